# Optimizing a Trainium2 kernel written in Bass

```python
import math
import jax, jax.numpy as jnp
from jax import lax
import numpy as np

D_MODEL = 2048
BATCH = 1
SEQ = 8192
DEPTH = 4

CHUNK = 64
N_A_LAYERS = DEPTH // 2
N_B_LAYERS = DEPTH - N_A_LAYERS
HEAD_DIM = 128
MEM_TOKENS = 256
MEM_HEADS = 4
MEM_W = MEM_HEADS * HEAD_DIM
MIX_W = D_MODEL - MEM_W
SGU_CHUNK = 128
SGU_GROUPS = 4
SGU_GROUP_W = MIX_W // SGU_GROUPS
DIFF_HEADS = MIX_W // (2 * HEAD_DIM)
DIFF_QK_W = 2 * DIFF_HEADS * HEAD_DIM
DIFF_V_DIM = 2 * HEAD_DIM
DIFF_BLOCK = 128
REL_BUCKETS = 32
REL_MAX_DIST = 128
N_GROUPS = 4
EXPERTS_PER_GROUP = 8
N_EXPERTS = N_GROUPS * EXPERTS_PER_GROUP
TOP_K = 2
EXPERT_FF = 512
MOE_BLOCK = 128
DN_ALPHA = (2 * DEPTH) ** 0.25
DN_BETA = (8 * DEPTH) ** -0.25
LN_EPS = 1e-5

kernel_name = 'hybrid_gmlp_diffattn_hmoe'


def layer_norm(x, g, b):
    xf = x.astype(jnp.float32)
    mu = jnp.mean(xf, -1, keepdims=True)
    var = jnp.mean(jnp.square(xf - mu), -1, keepdims=True)
    y = (xf - mu) * lax.rsqrt(var + LN_EPS)
    return (y * g.astype(jnp.float32) + b.astype(jnp.float32)).astype(x.dtype)


def rms_norm(x, g):
    xf = x.astype(jnp.float32)
    y = xf * lax.rsqrt(jnp.mean(jnp.square(xf), -1, keepdims=True) + LN_EPS)
    return (y * g.astype(jnp.float32)).astype(x.dtype)


def relative_bucket(rel):
    n = REL_BUCKETS // 2
    max_exact = n // 2
    ret = jnp.where(rel > 0, n, 0)
    a = jnp.abs(rel)
    af = jnp.maximum(a, 1).astype(jnp.float32)
    large = max_exact + (jnp.log(af / max_exact) / math.log(REL_MAX_DIST / max_exact)
                         * (n - max_exact)).astype(jnp.int32)
    large = jnp.minimum(large, n - 1)
    return ret + jnp.where(a < max_exact, a, large)


def spatial_gating(z, ln_g, ln_b, ws, bs):
    b_, s_, _ = z.shape
    u, v = jnp.split(z, 2, axis=-1)
    v = layer_norm(v, ln_g, ln_b)
    v = v.reshape(b_, s_ // SGU_CHUNK, SGU_CHUNK, SGU_GROUPS, SGU_GROUP_W)
    pos = jnp.arange(SGU_CHUNK)
    mask = (pos[None, :] // CHUNK) <= (pos[:, None] // CHUNK)
    w = jnp.where(mask[None], ws, jnp.zeros_like(ws))
    gate = jnp.einsum('gij,bnjgc->bnigc', w, v) + jnp.transpose(bs)[None, None, :, :, None]
    return u * gate.reshape(b_, s_, MIX_W)


def memory_attention(q, mem, w_kv):
    b_, s_, _ = q.shape
    kv = jnp.einsum('bmd,de->bme', mem, w_kv)
    k, v = jnp.split(kv, 2, axis=-1)
    q = q.reshape(b_, s_, MEM_HEADS, HEAD_DIM)
    k = k.reshape(b_, -1, MEM_HEADS, HEAD_DIM)
    v = v.reshape(b_, -1, MEM_HEADS, HEAD_DIM)
    s = jnp.einsum('bqhd,bkhd->bhqk', q, k).astype(jnp.float32) * (HEAD_DIM ** -0.5)
    p = jax.nn.softmax(s, axis=-1).astype(v.dtype)
    o = jnp.einsum('bhqk,bkhd->bqhd', p, v)
    return o.reshape(b_, s_, MEM_W)


def diff_attention(q, k, v, lam_params, subln_g, rel_bias, layer_idx):
    b_, s_, _ = q.shape
    lambda_init = 0.8 - 0.6 * math.exp(-0.3 * layer_idx)
    lp = lam_params.astype(jnp.float32)
    lam = jnp.exp(jnp.sum(lp[0] * lp[1])) - jnp.exp(jnp.sum(lp[2] * lp[3])) + lambda_init
    nqb = s_ // DIFF_BLOCK
    qb = q.reshape(b_, nqb, DIFF_BLOCK, DIFF_HEADS, 2, HEAD_DIM).transpose(1, 0, 2, 3, 4, 5)
    kpos = jnp.arange(s_)
    scale = HEAD_DIM ** -0.5

    def block(args):
        qblk, bi = args
        qpos = bi * DIFF_BLOCK + jnp.arange(DIFF_BLOCK)
        bias = rel_bias[relative_bucket(kpos[None, :] - qpos[:, None])]
        bias = jnp.transpose(bias, (2, 0, 1)).astype(jnp.float32)
        mask = (kpos[None, :] // CHUNK) <= (qpos[:, None] // CHUNK)
        s = jnp.einsum('bqhmd,bkhmd->bmhqk', qblk, k).astype(jnp.float32) * scale + bias
        s = jnp.where(mask, s, -jnp.inf)
        p = jax.nn.softmax(s, axis=-1)
        a = (p[:, 0] - lam * p[:, 1]).astype(v.dtype)
        return jnp.einsum('bhqk,bkhe->bqhe', a, v)

    o = lax.map(block, (qb, jnp.arange(nqb)))
    o = o.transpose(1, 0, 2, 3, 4).reshape(b_, s_, DIFF_HEADS, DIFF_V_DIM)
    o = rms_norm(o, subln_g) * (1.0 - lambda_init)
    return o.reshape(b_, s_, MIX_W)


def hierarchical_moe(x, wg1, bg1, wg2, bg2, w1, w3, w2):
    b_, s_, d_ = x.shape
    t = b_ * s_
    xt = x.reshape(t, d_)
    lg1 = jnp.einsum('td,dg->tg', xt, wg1).astype(jnp.float32) + bg1.astype(jnp.float32)
    pg = jax.nn.softmax(lg1, axis=-1)
    grp = jnp.argmax(lg1, axis=-1)
    p_sel = jnp.take_along_axis(pg, grp[:, None], axis=1)[:, 0]
    lg2_all = jnp.einsum('td,gde->tge', xt, wg2).astype(jnp.float32) + bg2.astype(jnp.float32)
    lg2 = jnp.take_along_axis(lg2_all, grp[:, None, None], axis=1)[:, 0]
    top_v, top_i = lax.top_k(lg2, TOP_K)
    gate = p_sel[:, None] * jax.nn.softmax(top_v, axis=-1)
    expert = grp[:, None] * EXPERTS_PER_GROUP + top_i
    n_assign = t * TOP_K
    flat_e = expert.reshape(-1)
    flat_w = gate.reshape(-1)
    flat_tok = jnp.repeat(jnp.arange(t, dtype=jnp.int32), TOP_K)
    order = jnp.argsort(flat_e)
    e_s, tok_s, w_s = flat_e[order], flat_tok[order], flat_w[order]
    counts = jnp.bincount(flat_e, length=N_EXPERTS)
    start = jnp.cumsum(counts) - counts
    padded = ((counts + MOE_BLOCK - 1) // MOE_BLOCK) * MOE_BLOCK
    pend = jnp.cumsum(padded)
    pstart = pend - padded
    dest = pstart[e_s] + (jnp.arange(n_assign) - start[e_s])
    n_blocks = n_assign // MOE_BLOCK + N_EXPERTS
    p_rows = n_blocks * MOE_BLOCK
    x_buf = jnp.zeros((p_rows, d_), x.dtype).at[dest].set(xt[tok_s])
    tok_buf = jnp.zeros((p_rows,), jnp.int32).at[dest].set(tok_s)
    w_buf = jnp.zeros((p_rows,), jnp.float32).at[dest].set(w_s)
    blk_e = jnp.minimum(jnp.searchsorted(pend, jnp.arange(n_blocks) * MOE_BLOCK, side='right'),
                        N_EXPERTS - 1)

    def expert_block(args):
        xb, e = args
        h = jax.nn.silu(xb @ w1[e]) * (xb @ w3[e])
        return h @ w2[e]

    yb = lax.map(expert_block, (x_buf.reshape(n_blocks, MOE_BLOCK, d_), blk_e)).reshape(p_rows, d_)
    y = jnp.zeros((t, d_), x.dtype).at[tok_buf].add(yb * w_buf[:, None].astype(yb.dtype))
    return y.reshape(b_, s_, d_)


def setup_inputs(seed: int = 0) -> dict:
    key = jax.random.key(seed)
    ks = jax.random.split(key, 24)
    f32 = jnp.float32

    def nrm(k, shape, scale):
        return jax.random.normal(k, shape, f32) * scale

    inv_d = D_MODEL ** -0.5
    return {
        'x': nrm(ks[0], (BATCH, SEQ, D_MODEL), 1.0),
        'mem': nrm(ks[1], (BATCH, MEM_TOKENS, D_MODEL), 1.0),
        'a_w_in': nrm(ks[2], (N_A_LAYERS, D_MODEL, 2 * MIX_W + MEM_W), inv_d),
        'a_sgu_ln_g': 1.0 + nrm(ks[3], (N_A_LAYERS, MIX_W), 0.02),
        'a_sgu_ln_b': nrm(ks[4], (N_A_LAYERS, MIX_W), 0.02),
        'a_ws': nrm(ks[5], (N_A_LAYERS, SGU_GROUPS, SGU_CHUNK, SGU_CHUNK), SGU_CHUNK ** -0.5),
        'a_bs': 1.0 + nrm(ks[6], (N_A_LAYERS, SGU_GROUPS, SGU_CHUNK), 0.1),
        'a_w_out': nrm(ks[7], (N_A_LAYERS, MIX_W + MEM_W, D_MODEL), (MIX_W + MEM_W) ** -0.5 * DN_BETA),
        'b_w_in': nrm(ks[8], (N_B_LAYERS, D_MODEL, DIFF_QK_W + MEM_W), inv_d),
        'b_lambda': nrm(ks[9], (N_B_LAYERS, 4, HEAD_DIM), 0.1),
        'b_subln_g': 1.0 + nrm(ks[10], (N_B_LAYERS, DIFF_V_DIM), 0.02),
        'b_w_out': nrm(ks[11], (N_B_LAYERS, MIX_W + MEM_W, D_MODEL), (MIX_W + MEM_W) ** -0.5 * DN_BETA),
        'shared_w_kv': nrm(ks[12], (D_MODEL, DIFF_QK_W + DIFF_HEADS * DIFF_V_DIM), inv_d),
        'rel_bias': nrm(ks[13], (REL_BUCKETS, DIFF_HEADS), 0.5),
        'mem_w_kv': nrm(ks[14], (DEPTH, D_MODEL, 2 * MEM_W), inv_d),
        'ln_g': 1.0 + nrm(ks[15], (DEPTH, 2, D_MODEL), 0.02),
        'ln_b': nrm(ks[16], (DEPTH, 2, D_MODEL), 0.02),
        'moe_wg1': nrm(ks[17], (DEPTH, D_MODEL, N_GROUPS), inv_d),
        'moe_bg1': nrm(ks[18], (DEPTH, N_GROUPS), 0.01),
        'moe_wg2': nrm(ks[19], (DEPTH, N_GROUPS, D_MODEL, EXPERTS_PER_GROUP), inv_d),
        'moe_bg2': nrm(ks[20], (DEPTH, N_GROUPS, EXPERTS_PER_GROUP), 0.01),
        'moe_w1': nrm(ks[21], (DEPTH, N_EXPERTS, D_MODEL, EXPERT_FF), inv_d),
        'moe_w3': nrm(ks[22], (DEPTH, N_EXPERTS, D_MODEL, EXPERT_FF), inv_d),
        'moe_w2': nrm(ks[23], (DEPTH, N_EXPERTS, EXPERT_FF, D_MODEL), EXPERT_FF ** -0.5 * DN_BETA),
    }


def reference(x, mem, a_w_in, a_sgu_ln_g, a_sgu_ln_b, a_ws, a_bs, a_w_out, b_w_in, b_lambda,
              b_subln_g, b_w_out, shared_w_kv, rel_bias, mem_w_kv, ln_g, ln_b, moe_wg1, moe_bg1,
              moe_wg2, moe_bg2, moe_w1, moe_w3, moe_w2):
    b_, s_, _ = x.shape
    shared_k = None
    shared_v = None
    for l in range(DEPTH):
        if l < N_A_LAYERS:
            i = l
            z = jnp.einsum('bsd,de->bse', x, a_w_in[i])
            z_mix, q_mem = z[..., :2 * MIX_W], z[..., 2 * MIX_W:]
            mix_out = spatial_gating(jax.nn.gelu(z_mix, approximate=False),
                                     a_sgu_ln_g[i], a_sgu_ln_b[i], a_ws[i], a_bs[i])
            w_out = a_w_out[i]
        else:
            i = l - N_A_LAYERS
            z = jnp.einsum('bsd,de->bse', x, b_w_in[i])
            q_diff, q_mem = z[..., :DIFF_QK_W], z[..., DIFF_QK_W:]
            mix_out = diff_attention(q_diff, shared_k, shared_v, b_lambda[i], b_subln_g[i], rel_bias, l)
            w_out = b_w_out[i]
        mem_out = memory_attention(q_mem, mem, mem_w_kv[l])
        t = jnp.einsum('bse,ed->bsd', jnp.concatenate([mix_out, mem_out], axis=-1), w_out)
        x = layer_norm(DN_ALPHA * x + t, ln_g[l, 0], ln_b[l, 0])
        f = hierarchical_moe(x, moe_wg1[l], moe_bg1[l], moe_wg2[l], moe_bg2[l],
                             moe_w1[l], moe_w3[l], moe_w2[l])
        x = layer_norm(DN_ALPHA * x + f, ln_g[l, 1], ln_b[l, 1])
        if l == N_A_LAYERS - 1:
            kv = jnp.einsum('bsd,de->bse', x, shared_w_kv)
            shared_k = kv[..., :DIFF_QK_W].reshape(b_, s_, DIFF_HEADS, 2, HEAD_DIM)
            shared_v = kv[..., DIFF_QK_W:].reshape(b_, s_, DIFF_HEADS, DIFF_V_DIM)
    return x
```

```python
import math
import numpy as np
import ml_dtypes
from contextlib import ExitStack
from concourse.bass_utils import run_bass_kernel_spmd

import concourse.bass as bass
import concourse.mybir as mybir

F32 = mybir.dt.float32
BF16 = mybir.dt.bfloat16
U32 = mybir.dt.uint32
I32 = mybir.dt.int32
AF = mybir.ActivationFunctionType
ALU = mybir.AluOpType
AX = mybir.AxisListType

ENGS = ("pe", "act", "dve", "pool", "sp")


class Buf:
    __slots__ = ("name", "writes", "reads", "sem", "semval", "base")

    def __init__(self, name):
        self.name = name
        self.writes = []
        self.reads = []
        self.base = None
        self.sem = None
        self.semval = 0


class _Rec:
    def __init__(self):
        self.call = None

    def __getattr__(self, name):
        def f(*a, **k):
            self.call = (name, a, k)
            return self
        return f


class Ins:
    __slots__ = ("eng", "fn", "waits", "flag", "seq", "semval", "dma", "noncontig")

    def __init__(self, eng, fn):
        self.eng = eng
        self.noncontig = False
        if fn is not None:
            rec = _Rec()
            fn(rec)
            assert rec.call is not None
            self.fn = rec.call
        else:
            self.fn = None
        self.waits = []
        self.flag = False
        self.seq = 0
        self.semval = None
        self.dma = None


class Prog:
    def __init__(self, nc, stack, same_engine_sync=True):
        self.nc = nc
        self.stack = stack
        self.lists = {e: [] for e in ENGS}
        self.esem = {e: stack.enter_context(nc.semaphore("es_" + e)) for e in ENGS}
        self.waited_eng = {e: {} for e in ENGS}
        self.waited_dma = {e: {} for e in ENGS}
        self.same = same_engine_sync
        self.nbuf = 0
        self.dma_sems = []

    def buf(self, name=None):
        self.nbuf += 1
        return Buf(name or "b%d" % self.nbuf)

    def share(self, bufs):
        sem = self.stack.enter_context(self.nc.semaphore("dg%d_%s" % (len(self.dma_sems), bufs[0].name)))
        self.dma_sems.append(sem)
        g = [sem, 0]
        for b in bufs:
            b.sem = g

    def fence(self, bufs):
        for e in ENGS:
            self.final_wait(e, bufs)

    def bufs(self, n, name="b"):
        return [self.buf("%s%d" % (name, i)) for i in range(n)]

    def _add_wait(self, ins, ev):
        e = ins.eng
        if ev[0] == "c":
            src = ev[1]
            if src.eng == e and (e == "pe" or not self.same):
                return
            w = self.waited_eng[e]
            if w.get(src.eng, -1) >= src.seq:
                return
            w[src.eng] = src.seq
            src.flag = True
            ins.waits.append(ev)
        else:
            _, sem, val = ev
            w = self.waited_dma[e]
            k = id(sem)
            if w.get(k, -1) >= val:
                return
            w[k] = val
            ins.waits.append(ev)

    def emit(self, eng, fn, reads=(), writes=(), cwrites=(), dma_dst=None, noncontig=False):
        ins = Ins(eng, fn)
        ins.noncontig = noncontig
        lst = self.lists[eng]
        ins.seq = len(lst)
        deps = []
        for b in reads:
            deps.extend(b.writes)
        for b in writes:
            deps.extend(b.writes)
            deps.extend(b.reads)
        for b in cwrites:
            deps.extend(b.reads)
            if b.base is not None:
                deps.append(b.base)
        if len(deps) > 1:
            deps = self._compact(deps)
        for ev in deps:
            self._add_wait(ins, ev)
        if dma_dst is not None:
            if dma_dst.sem is None:
                sem = self.stack.enter_context(self.nc.semaphore("ds%d_%s" % (len(self.dma_sems), dma_dst.name)))
                self.dma_sems.append(sem)
                dma_dst.sem = [sem, 0]
            g = dma_dst.sem
            g[1] += 16
            ins.dma = (g[0], g[1])
            ev = ("d", g[0], g[1])
        else:
            ev = ("c", ins)
        for b in writes:
            b.writes = [ev]
            b.reads = []
            b.base = ev
        for b in cwrites:
            b.writes.append(ev)
            if len(b.writes) > 24:
                b.writes = self._compact(b.writes)
        for b in reads:
            b.reads.append(ev)
            if len(b.reads) > 24:
                b.reads = self._compact(b.reads)
        lst.append(ins)
        return ins

    @staticmethod
    def _compact(evs):
        best = {}
        for ev in evs:
            if ev[0] == "c":
                k = ("c", ev[1].eng)
                if k not in best or best[k][1].seq < ev[1].seq:
                    best[k] = ev
            else:
                k = ("d", id(ev[1]))
                if k not in best or best[k][2] < ev[2]:
                    best[k] = ev
        return list(best.values())

    def barrier(self, bufs):
        pass

    def final_wait(self, eng, bufs):
        ins = Ins(eng, None)
        ins.seq = len(self.lists[eng])
        for b in bufs:
            for ev in b.writes:
                self._add_wait(ins, ev)
        self.lists[eng].append(ins)

    def finalize(self):
        nc = self.nc
        for e in ENGS:
            c = 0
            for ins in self.lists[e]:
                if ins.flag:
                    c += 1
                    ins.semval = c
        engobj = {"pe": "tensor", "act": "scalar", "dve": "vector", "pool": "gpsimd", "sp": "sync"}
        stats = {}
        with nc.Block() as block:
            for e in ENGS:
                lst = self.lists[e]
                esem = self.esem

                def body(eng, lst=lst, e=e):
                    nw = 0
                    for ins in lst:
                        for ev in ins.waits:
                            if ev[0] == "c":
                                eng.wait_ge(esem[ev[1].eng], ev[1].semval)
                            else:
                                eng.wait_ge(ev[1], ev[2])
                            nw += 1
                        if ins.fn is None:
                            continue
                        name, a, k = ins.fn
                        if ins.noncontig:
                            with nc.allow_non_contiguous_dma(reason="tiny strided load"):
                                r = getattr(eng, name)(*a, **k)
                        else:
                            r = getattr(eng, name)(*a, **k)
                        if ins.dma is not None:
                            r.then_inc(ins.dma[0], 16)
                            assert not ins.flag
                        elif ins.flag:
                            r.then_inc(esem[e], 1)
                    stats[e] = (len(lst), nw)

                getattr(block, engobj[e])(body)
        return stats


NCORES = 8
T = 8
NT = 1024
D = 2048
CAP = 128
NSLOT = 32 * CAP
ALPHA = float(8 ** 0.25)
EPS = 1e-5
SCALE = float(128 ** -0.5)
NEG = -30000.0


def zigzag_blocks(c):
    out = []
    for p in range(4):
        out.append(16 * p + c)
        out.append(16 * p + 15 - c)
    return out


GBLK = [zigzag_blocks(c) for c in range(NCORES)]
LT = [max(GBLK[c][t] for c in range(NCORES)) + 1 for t in range(T)]
OWNER = {}
for _c in range(NCORES):
    for _t in range(T):
        OWNER[GBLK[_c][_t]] = (_c, _t)


class Ctx:
    def __init__(self, nc, st):
        self.nc, self.st = nc, st
        self.P = Prog(nc, st)
        self.ps = [st.enter_context(nc.psum_tensor("ps%d" % i, [128, 512], F32)) for i in range(8)]
        self.bps = self.P.bufs(8, "ps")
        self.psi = 0
        self.evi = 0
        self.tmpi = 0

    def next_ps(self):
        i = self.psi
        self.psi = (i + 1) % 8
        return self.ps[i], self.bps[i]

    def sb(self, name, shape, dt):
        return self.st.enter_context(self.nc.sbuf_tensor(name, shape, dt))

    def dram(self, name, shape, dt, kind="Internal"):
        return self.nc.dram_tensor(name, shape, dt, kind=kind).ap()

    def E(self, eng, fn, r=(), w=(), cw=(), dma=None, nonc=False):
        return self.P.emit(eng, fn, reads=r, writes=w, cwrites=cw, dma_dst=dma, noncontig=nonc)

    def evac(self, out, in_, r, w=(), cw=()):
        self.evi += 1
        if self.evi % 2 == 0:
            self.E("dve", lambda e: e.tensor_copy(out=out, in_=in_), r, w, cw)
        else:
            self.E("act", lambda e: e.activation(out=out, in_=in_, func=AF.Copy), r, w, cw)


def build_program(mode, nlayers=2, debug=False):
    nc = bass.Bass("TRN2", target_bir_lowering=False)
    st = ExitStack()
    c = Ctx(nc, st)
    P = c.P
    E = c.E
    IN = lambda name, shape, dt=F32: nc.dram_tensor(name, shape, dt, kind="ExternalInput").ap()
    OUT = lambda name, shape, dt=F32: nc.dram_tensor(name, shape, dt, kind="ExternalOutput").ap()

    x_in = IN("x", [NT, D])
    mem = IN("mem", [256, D])
    consts = IN("consts", [128, 448])
    fused = (mode == "F")
    NL = 4 if fused else 2
    ln_g = IN("ln_g", [NL, 2, D])
    ln_b = IN("ln_b", [NL, 2, D])
    mem_w_kv = IN("mem_w_kv", [NL, D, 1024])
    moe_wr = IN("moe_wr", [NL, D, 36])
    moe_br = IN("moe_br", [NL, 36])
    _w1 = [IN("moe_w1_%d" % i, [2, 32, D, 512]) for i in range(NL // 2)]
    _w3 = [IN("moe_w3_%d" % i, [2, 32, D, 512]) for i in range(NL // 2)]
    _w2 = [IN("moe_w2_%d" % i, [2, 32, 512, D]) for i in range(NL // 2)]

    class _Split:
        def __init__(self, parts):
            self.parts = parts

        def __getitem__(self, key):
            l_, ex_ = key
            return self.parts[l_ // 2][l_ % 2, ex_]

    moe_w1, moe_w3, moe_w2 = _Split(_w1), _Split(_w3), _Split(_w2)
    if mode in ("A", "F"):
        a_w_in = IN("a_w_in", [2, D, 3584])
        a_sgu_ln_g = IN("a_sgu_ln_g", [2, 1536])
        a_sgu_ln_b = IN("a_sgu_ln_b", [2, 1536])
        a_ws = IN("a_ws", [2, 4, 128, 128])
        a_bs = IN("a_bs", [2, 4, 128])
        a_w_out = IN("a_w_out", [2, D, D])
        shared_w_kv = IN("shared_w_kv", [D, 3072])
        y_out = OUT("y", [NT, D])
        if not fused:
            kt_out = OUT("kt", [T, 6, 128, 2, 128], BF16)
            v_out = OUT("v", [T, 128, 6, 257], BF16)
        layers = [0, 1][:nlayers]
        if debug:
            dbg_slots = OUT("dbg_slots", [128, T, 2], U32)
            dbg_gates = OUT("dbg_gates", [128, T, 2], F32)
            dbg_xbuf = OUT("dbg_xbuf", [NSLOT + 128, D], BF16)
            dbg_ybuf = OUT("dbg_ybuf", [NSLOT + 128, D], BF16)
            dbg_xm = OUT("dbg_xm", [NT, D], F32)
            dbg_catT = OUT("dbg_catT", [128, 16, NT], BF16)
            dbg_vv = OUT("dbg_vv", [128, T * 1536], BF16)
    if mode in ("B", "F"):
        b_w_in = IN("b_w_in", [2, D, D])
        b_lambda = IN("b_lambda", [2, 4, 128])
        b_subln_g = IN("b_subln_g", [2, 256])
        b_w_out = IN("b_w_out", [2, D, D])
        rel_bias = IN("rel_bias", [32, 6])
        if fused:
            ktl2 = c.dram("ktl2", [T * 6 * 128, 256], BF16)
            vl2 = c.dram("vl2", [T * 6 * 128, 257], BF16)
            gk2 = c.dram("gk2", [NCORES * T * 6 * 128, 256], BF16)
            gv2 = c.dram("gv2", [NCORES * T * 6 * 128, 257], BF16)
            kt_out = ktl2.rearrange("(t h d) (m k) -> t h d m k", t=T, h=6, m=2)
            vl_v = vl2.rearrange("(t h k) e -> t h k e", t=T, h=6)
            g_k = gk2.rearrange("(r t h d) (m k) -> r t h d m k", r=NCORES, t=T, h=6, m=2)
            g_v = gv2.rearrange("(r t h k) e -> r t h k e", r=NCORES, t=T, h=6)
            previdx = IN("previdx", [128, T, 6], U32)
            nearmask = IN("nearmask", [128, T])
        else:
            g_k = IN("g_k", [6, 128, 64, 2, 128], BF16)
            g_v = IN("g_v", [6, 128, 64, 257], BF16)
            near_k = IN("near_k", [T, 6, 128, 2, 2, 128], BF16)
            near_v = IN("near_v", [T, 6, 128, 2, 257], BF16)
            y_out = OUT("y", [NT, D])
        farmask = IN("farmask", [128, T, 64])
        bidx = IN("bidx", [128, 2, 128])
        layers = [0, 1, 2, 3] if fused else [2, 3][:nlayers]
        if debug:
            dbg_catT = OUT("dbg_catT", [128, 16, NT], BF16)
            dbg_tbl = OUT("dbg_tbl", [128, 1536], BF16)
            dbg_qh = OUT("dbg_qh", [128, 2048], BF16)
            dbg_lam = OUT("dbg_lam", [128, 8], F32)

    L0 = layers[0]
    xm_d = c.dram("xm_d", [NT, D], F32)
    x_d = c.dram("x_d", [NT, D], F32)
    xbuf = c.dram("xbuf", [NSLOT + 128, D], BF16)
    ybuf = c.dram("ybuf", [NSLOT + 128, D], BF16)
    b_xm_d = P.bufs(T, "xmd")
    b_x_d = P.bufs(T, "xd")
    b_xbuf = P.buf("xbuf")
    b_ybuf = P.bufs(32, "ybuf")
    b_y = P.bufs(T, "yout")
    for grp in (b_xm_d, b_x_d, b_y):
        P.share(grp[0::2])
        P.share(grp[1::2])
    P.share(b_ybuf[0::2])
    P.share(b_ybuf[1::2])

    pool = c.sb("wpool", [128, 6 * 8192], BF16)
    bw = P.bufs(6, "slab")

    def slab(i):
        return pool[:, i * 8192:(i + 1) * 8192]

    def slab_in(i):
        return slab(i).rearrange("p (k n) -> p k n", k=16)

    def slab_w2(i):
        return slab(i).rearrange("p (k n) -> p k n", k=4)

    xT = pool[:, 2 * 8192:4 * 8192].rearrange("p (k n) -> p k n", k=16)
    catT = pool[:, 4 * 8192:6 * 8192].rearrange("p (k n) -> p k n", k=16)
    b_xT = P.bufs(T, "xT")
    b_catT = P.bufs(T, "catT")
    XG = [bw[2], bw[3]]
    CG = [bw[4], bw[5]]

    cst = c.sb("cst", [128, 448], F32)
    b_cst = P.buf("cst")
    identb = c.sb("identb", [128, 128], BF16)
    ustrb = c.sb("ustrb", [128, 128], BF16)
    onesb = c.sb("onesb", [128, 128], BF16)
    b_cb = P.buf("cb")
    identf = cst[:, 0:128]
    sgumask = cst[:, 256:384]
    ebase = cst[:, 384:416]
    trashc = cst[:, 416:417]

    gB = c.sb("gB", [128, D], F32)
    bB = c.sb("bB", [128, D], F32)
    b_gB = P.buf("gB")
    xw = [c.sb("xw%d" % i, [128, D], F32) for i in range(2)]
    b_xw = P.bufs(2, "xw")
    kmT = c.sb("kmT", [128, 4, 256], BF16)
    vm = c.sb("vm", [128, 2, 4, 129], BF16)
    b_kmT = P.buf("kmT")
    b_vm = P.buf("vm")
    qmT = c.sb("qmT", [128, 4, NT], BF16)
    b_qmT = P.buf("qmT")
    stt = c.sb("stt", [128, T, 4, 6], F32)
    mvt = c.sb("mvt", [128, T, 2], F32)
    rst = c.sb("rst", [128, T, 4], F32)
    b_st = P.bufs(T, "st")
    wr = c.sb("wr", [128, 16, 36], F32)
    brB = c.sb("brB", [128, 36], F32)
    b_wr = P.buf("wr")
    xTf_t = c.sb("xTf", [128, 16, 128], F32)
    xTf = [xTf_t, xTf_t]
    _bx = P.buf("xTf")
    b_xTf = [_bx, _bx]
    rt = c.sb("rt", [128, T, 160], F32)
    b_rt = P.bufs(T, "rt")
    gates = c.sb("gates", [128, T, 2], F32)
    slots = c.sb("slots", [128, T, 2], U32)
    Ab = c.sb("Ab", [128, T, 32], BF16)
    b_Ab = P.bufs(T, "Ab")
    b_gs = P.bufs(T, "gs")
    xbf_t = c.sb("xbf", [128, D], BF16)
    xbf = [xbf_t, xbf_t]
    _bx2 = P.buf("xbf")
    b_xbf = [_bx2, _bx2]
    c.vreg = c.sb("vreg", [128, 16896], BF16)
    vr = c.vreg
    xe = [vr[:, i * 2048:(i + 1) * 2048] for i in range(2)]
    b_xe = P.bufs(2, "xe")
    xeT = [vr[:, 4096 + i * 2048:4096 + (i + 1) * 2048].rearrange("p (k n) -> p k n", k=16) for i in range(2)]
    b_xeT = P.bufs(2, "xeT")
    hs = [vr[:, 8192 + i * 1024:8192 + (i + 1) * 1024].bitcast(F32) for i in range(2)]
    hb = [vr[:, 10240 + i * 512:10240 + (i + 1) * 512] for i in range(2)]
    hT = [vr[:, 11264 + i * 512:11264 + (i + 1) * 512].rearrange("p (k n) -> p k n", k=4) for i in range(2)]
    b_hs = P.bufs(2, "hs")
    b_hb = P.bufs(2, "hb")
    b_hT = P.bufs(2, "hT")
    ye = [vr[:, 12288 + i * 2048:12288 + (i + 1) * 2048] for i in range(2)]
    b_ye = P.bufs(2, "ye")
    y01_t = xTf_t[:].rearrange("p a b -> p (a b)").bitcast(BF16).rearrange("p (a b) -> p a b", a=2)
    y01 = [y01_t, y01_t]
    _by = P.buf("y01")
    b_y01 = [_by, _by]
    catc = [c.sb("catc%d" % i, [128, 512], BF16) for i in range(3)]
    b_catc = P.bufs(3, "catc")
    pT = [c.sb("pT%d" % i, [128, 512], BF16) for i in range(3)]
    b_pT = P.bufs(3, "pT")
    rc = c.sb("rc", [128, 16], F32)
    b_rc = P.bufs(16, "rc")
    cnt = {"catc": 0, "pT": 0, "rc": 0, "ug": 0}

    def rot(name, n):
        i = cnt[name]
        cnt[name] = (i + 1) % n
        return i

    E("sp", lambda e: e.dma_start(out=cst[:], in_=consts), w=[b_cst], dma=b_cst)
    E("dve", lambda e: e.tensor_copy(out=identb[:], in_=cst[:, 0:128]), r=[b_cst], w=[b_cb])
    E("dve", lambda e: e.tensor_copy(out=ustrb[:], in_=cst[:, 128:256]), r=[b_cst], cw=[b_cb])
    E("dve", lambda e: e.memset(onesb[:], 1.0), cw=[b_cb])
    E("dve", lambda e: e.memset(vm[:, :, :, 128:129], 1.0), w=[b_vm])

    E("dve", lambda e: e.memset(ye[0], 0.0), w=[b_ye[0]])
    E("sp", lambda e: e.dma_start(out=ybuf[NSLOT:NSLOT + 128, :], in_=ye[0]), r=[b_ye[0]], w=[b_ybuf[0]], dma=b_ybuf[0])

    def transpose_tile_f32(src, b_src, dst_fn, b_dst_w, b_dst_cw=(), dst_dt_copy=True):
        for g in range(4):
            ps, bp = c.next_ps()
            for j in range(4):
                k = 4 * g + j
                E("pe", lambda e, ps=ps, j=j, k=k: e.transpose(out=ps[:, j * 128:(j + 1) * 128],
                                                          in_=src[:, k * 128:(k + 1) * 128], identity=identf),
                  r=[b_src, b_cst], w=[bp] if j == 0 else (), cw=() if j == 0 else [bp])
            c.evac(dst_fn(g), ps[:].rearrange("p (a b) -> p a b", a=4), r=[bp],
                   w=b_dst_w if g == 0 else (), cw=list(b_dst_cw) + (list(b_dst_w) if g else []))

    def make_xT(src, b_src, t):
        transpose_tile_f32(src, b_src, lambda g, t=t: xT[:, 4 * g:4 * g + 4, t * 128:(t + 1) * 128],
                           [b_xT[t]], XG)

    if True:
        for t in range(T):
            i = t % 2
            E("sp", lambda e, i=i, t=t: e.dma_start(out=xw[i][:], in_=x_in[t * 128:(t + 1) * 128, :]),
              w=[b_xw[i]], dma=b_xw[i])
            make_xT(xw[i], b_xw[i], t)

    def load_slab_in(i, W, c0, ncols=512):
        dst = slab(i)[:, 0:16 * ncols].rearrange("p (k n) -> p k n", k=16)
        E("pool", lambda e: e.dma_start(out=dst, in_=W[:, c0:c0 + ncols].rearrange("(k p) n -> p k n", p=128)),
          w=[bw[i]], dma=bw[i])
        return dst

    def bcast_load(dst, vec, b_dst, n):
        E("sp", lambda e: e.dma_start(out=dst, in_=vec.partition_broadcast(128)), w=[b_dst], dma=b_dst)

    def ln_tile(xt, b_xt, t, width, gap, bap, b_gb, out_ap=None, b_out=None, sti=0):
        nch = width // 512
        for j in range(nch):
            E("dve", lambda e, j=j: e.bn_stats(out=stt[:, t, j, :], in_=xt[:, j * 512:(j + 1) * 512]),
              r=[b_xt], w=[b_st[t]] if j == 0 else (), cw=() if j == 0 else [b_st[t]])
        E("dve", lambda e: e.bn_aggr(out=mvt[:, t, :], in_=stt[:, t, 0:nch, :]), r=[b_st[t]], cw=[b_st[t]])
        E("dve", lambda e: e.tensor_scalar(out=rst[:, t, 0:1], in0=mvt[:, t, 1:2], scalar1=EPS, scalar2=None,
                                           op0=ALU.add), r=[b_st[t]], cw=[b_st[t]])
        E("act", lambda e: e.activation(out=rst[:, t, 1:2], in_=rst[:, t, 0:1], func=AF.Ln), r=[b_st[t]], cw=[b_st[t]])
        E("act", lambda e: e.activation(out=rst[:, t, 2:3], in_=rst[:, t, 1:2], func=AF.Exp, scale=-0.5), r=[b_st[t]], cw=[b_st[t]])
        E("dve", lambda e: e.tensor_scalar(out=rst[:, t, 3:4], in0=mvt[:, t, 0:1], scalar1=rst[:, t, 2:3], scalar2=-1.0,
                                           op0=ALU.mult, op1=ALU.mult), r=[b_st[t]], cw=[b_st[t]])
        o = xt if out_ap is None else out_ap
        E("act", lambda e: e.activation(out=xt, in_=xt, func=AF.Identity, scale=rst[:, t, 2:3], bias=rst[:, t, 3:4]),
          r=[b_st[t]], w=[b_xt])
        E("dve", lambda e: e.tensor_tensor(out=xt, in0=xt, in1=gap, op=ALU.mult), r=[b_gb], w=[b_xt])
        if out_ap is None:
            E("dve", lambda e: e.tensor_tensor(out=xt, in0=xt, in1=bap, op=ALU.add), r=[b_gb], w=[b_xt])
        else:
            E("dve", lambda e: e.tensor_tensor(out=out_ap, in0=xt, in1=bap, op=ALU.add), r=[b_gb, b_xt], w=[b_out])

    def mem_kv(l):
        sK = load_slab_in(0, mem_w_kv[l - L0], 0)
        sV = load_slab_in(1, mem_w_kv[l - L0], 512)
        memT = xw[1][:].bitcast(BF16).rearrange("p (k n) -> p k n", k=16)
        b_memT = b_xw[1]
        for kc in range(2):
            E("sp", lambda e, kc=kc: e.dma_start(out=xw[0][:], in_=mem[kc * 128:(kc + 1) * 128, :]), w=[b_xw[0]], dma=b_xw[0])
            transpose_tile_f32(xw[0], b_xw[0], lambda g, kc=kc: memT[:, 4 * g:4 * g + 4, kc * 128:(kc + 1) * 128],
                               [b_memT] if kc == 0 else [], [] if kc == 0 else [b_memT])
        for h in range(4):
            ps, bp = c.next_ps()
            for k in range(16):
                E("pe", lambda e, ps=ps, k=k, h=h: e.matmul(ps[:, 0:256], lhsT=sK[:, k, h * 128:(h + 1) * 128],
                                                          rhs=memT[:, k, :], start=(k == 0), stop=(k == 15)),
                  r=[bw[0], b_memT], w=[bp] if k == 0 else (), cw=() if k == 0 else [bp])
            c.evac(kmT[:, h, :], ps[:, 0:256], r=[bp], w=[b_kmT] if h == 0 else (), cw=() if h == 0 else [b_kmT])
        for kc in range(2):
            ps, bp = c.next_ps()
            for k in range(16):
                E("pe", lambda e, ps=ps, k=k, kc=kc: e.matmul(ps[:], lhsT=memT[:, k, kc * 128:(kc + 1) * 128],
                                                            rhs=sV[:, k, :], start=(k == 0), stop=(k == 15)),
                  r=[bw[1], b_memT], w=[bp] if k == 0 else (), cw=() if k == 0 else [bp])
            c.evac(vm[:, kc, :, 0:128], ps[:].rearrange("p (h d) -> p h d", h=4), r=[bp], cw=[b_vm])

    def q_featmajor(sl, b_sl, ncol_chunks, dstfn, b_dst):
        first = True
        for fc in range(ncol_chunks):
            for half in range(2):
                ps, bp = c.next_ps()
                for k in range(16):
                    E("pe", lambda e, ps=ps, k=k, fc=fc, half=half: e.matmul(
                        ps[:], lhsT=sl[:, k, fc * 128:(fc + 1) * 128], rhs=xT[:, k, half * 512:(half + 1) * 512],
                        start=(k == 0), stop=(k == 15)),
                      r=[b_sl] + b_xT[4 * half:4 * half + 4] + XG, w=[bp] if k == 0 else (), cw=() if k == 0 else [bp])
                c.evac(dstfn(fc, half), ps[:], r=[bp], w=[b_dst] if first else (), cw=() if first else [b_dst])
                first = False

    def mem_attn_tile(t):
        ci = rot("catc", 3)
        cc, bcc = catc[ci], b_catc[ci]
        for h in range(4):
            ps, bp = c.next_ps()
            for kc in range(2):
                E("pe", lambda e, ps=ps, kc=kc, h=h: e.matmul(ps[:, kc * 128:(kc + 1) * 128],
                                                            lhsT=kmT[:, h, kc * 128:(kc + 1) * 128],
                                                            rhs=qmT[:, h, t * 128:(t + 1) * 128], start=True, stop=True),
                  r=[b_kmT, b_qmT], w=[bp] if kc == 0 else (), cw=() if kc == 0 else [bp])
            pi = rot("pT", 3)
            E("act", lambda e, ps=ps, pi=pi: e.activation(out=pT[pi][:, 0:256], in_=ps[:, 0:256], func=AF.Exp, scale=SCALE),
              r=[bp], w=[b_pT[pi]])
            ps2, bp2 = c.next_ps()
            for kc in range(2):
                E("pe", lambda e, ps2=ps2, kc=kc, h=h, pi=pi: e.matmul(ps2[:, 0:129], lhsT=pT[pi][:, kc * 128:(kc + 1) * 128],
                                                                     rhs=vm[:, kc, h, :], start=(kc == 0), stop=(kc == 1)),
                  r=[b_pT[pi], b_vm], w=[bp2] if kc == 0 else (), cw=() if kc == 0 else [bp2])
            ri = rot("rc", 16)
            E("dve", lambda e, ps2=ps2, ri=ri: e.reciprocal(out=rc[:, ri:ri + 1], in_=ps2[:, 128:129]), r=[bp2], w=[b_rc[ri]])
            E("dve", lambda e, ps2=ps2, ri=ri, h=h: e.tensor_scalar(out=cc[:, h * 128:(h + 1) * 128], in0=ps2[:, 0:128],
                                                                    scalar1=rc[:, ri:ri + 1], scalar2=None, op0=ALU.mult),
              r=[bp2, b_rc[ri]], w=[bcc] if h == 0 else (), cw=() if h == 0 else [bcc])
        cat_chunk_to_catT(cc, bcc, 12, t)

    def cat_chunk_to_catT(cc, bcc, k0, t, n=4):
        ps, bp = c.next_ps()
        psb = ps[:].bitcast(BF16)
        for j in range(n):
            E("pe", lambda e, j=j: e.transpose(out=psb[:, j * 128:(j + 1) * 128], in_=cc[:, j * 128:(j + 1) * 128],
                                               identity=identb[:]),
              r=[bcc, b_cb], w=[bp] if j == 0 else (), cw=() if j == 0 else [bp])
        c.evac(catT[:, k0:k0 + n, t * 128:(t + 1) * 128], psb[:, 0:n * 128].rearrange("p (a b) -> p a b", a=n),
               r=[bp], cw=[b_catT[t]] + CG)

    def mixer_A(l):
        i = l
        W = a_w_in[i]
        vv = c.vreg[:, 0:T * 1536].rearrange("p (t n) -> p t n", t=T)
        gS = gB[:, 0:1536]
        bS = bB[:, 0:1536]
        bcast_load(gS, a_sgu_ln_g[i], b_gB, 1536)
        E("sp", lambda e: e.dma_start(out=bS, in_=a_sgu_ln_b[i].partition_broadcast(128)), cw=[b_gB], dma=b_gB)
        wsf = xw[0][:, 0:512].rearrange("p (g j) -> p g j", g=4)
        E("sp", lambda e: e.dma_start(out=wsf, in_=a_ws[i].rearrange("g i j -> i g j")), w=[b_xw[0]], dma=b_xw[0])
        E("dve", lambda e: e.tensor_tensor(out=wsf, in0=wsf, in1=sgumask.unsqueeze(1).to_broadcast([128, 4, 128]),
                                           op=ALU.mult), r=[b_cst], w=[b_xw[0]])
        ps, bp = c.next_ps()
        for g in range(4):
            E("pe", lambda e, g=g: e.transpose(out=ps[:, g * 128:(g + 1) * 128], in_=xw[0][:, g * 128:(g + 1) * 128],
                                               identity=identf), r=[b_xw[0], b_cst],
              w=[bp] if g == 0 else (), cw=() if g == 0 else [bp])
        c.evac(c.wsT[:], ps[:].rearrange("p (g i) -> p g i", g=4), r=[bp], w=[c.b_wsT])
        E("sp", lambda e: e.dma_start(out=c.bsT[:], in_=a_bs[i].rearrange("g i -> i g")), w=[c.b_bsT], dma=c.b_bsT, nonc=True)

        for s in (3, 4, 5):
            sl = load_slab_in((s - 3) % 2, W, s * 512)
            for t in range(T):
                ps, bp = c.next_ps()
                for k in range(16):
                    E("pe", lambda e, ps=ps, k=k, t=t, sl=sl: e.matmul(ps[:], lhsT=xT[:, k, t * 128:(t + 1) * 128], rhs=sl[:, k, :],
                                                                      start=(k == 0), stop=(k == 15)),
                      r=[bw[(s - 3) % 2], b_xT[t]] + XG, w=[bp] if k == 0 else (), cw=() if k == 0 else [bp])
                E("act", lambda e, ps=ps, t=t, s=s: e.activation(out=vv[:, t, (s - 3) * 512:(s - 2) * 512], in_=ps[:], func=AF.Gelu),
                  r=[bp], w=[c.b_v[t]] if s == 3 else (), cw=() if s == 3 else [c.b_v[t]])
        sl = load_slab_in(1, W, 3072)
        q_featmajor(sl, bw[1], 4, lambda fc, half: qmT[:, fc, half * 512:(half + 1) * 512], b_qmT)
        for t in range(T):
            tmp = xw[t % 2][:, 0:1536]
            btmp = b_xw[t % 2]
            E("dve", lambda e, t=t, tmp=tmp: e.tensor_copy(out=tmp, in_=vv[:, t, :]), r=[c.b_v[t]], w=[btmp])
            ln_tile(tmp, btmp, t, 1536, gS, bS, b_gB, out_ap=vv[:, t, :], b_out=c.b_v[t])
        mem_kv(l)
        slabs_u = {0: 0, 1: 1, 2: 0}
        for s in (0, 1, 2):
            si = slabs_u[s]
            sl = load_slab_in(si, W, s * 512)
            for t in range(T):
                ps, bp = c.next_ps()
                for k in range(16):
                    E("pe", lambda e, ps=ps, k=k, t=t, sl=sl: e.matmul(ps[:], lhsT=xT[:, k, t * 128:(t + 1) * 128], rhs=sl[:, k, :],
                                                                      start=(k == 0), stop=(k == 15)),
                      r=[bw[si], b_xT[t]] + XG, w=[bp] if k == 0 else (), cw=() if k == 0 else [bp])
                ui = rot("pT", 3)
                ug = pT[ui]
                E("act", lambda e, ps=ps, ug=ug: e.activation(out=ug[:], in_=ps[:], func=AF.Gelu), r=[bp], w=[b_pT[ui]])
                psg, bpg = c.next_ps()
                ci = rot("catc", 3)
                cc, bcc = catc[ci], b_catc[ci]
                segs = []
                lo = s * 512
                while lo < (s + 1) * 512:
                    g = lo // 384
                    hi = min((g + 1) * 384, (s + 1) * 512)
                    segs.append((g, lo, hi))
                    lo = hi
                for n_, (g, lo, hi) in enumerate(segs):
                    E("pe", lambda e, psg=psg, g=g, lo=lo, hi=hi, t=t: e.matmul(psg[:, lo - s * 512:hi - s * 512], lhsT=c.wsT[:, g, :],
                                                                                rhs=vv[:, t, lo:hi], start=True, stop=True),
                      r=[c.b_wsT, c.b_v[t]], w=[bpg] if n_ == 0 else (), cw=() if n_ == 0 else [bpg])
                for n_, (g, lo, hi) in enumerate(segs):
                    E("dve", lambda e, psg=psg, g=g, lo=lo, hi=hi, cc=cc, ug=ug: e.scalar_tensor_tensor(
                        out=cc[:, lo - s * 512:hi - s * 512], in0=psg[:, lo - s * 512:hi - s * 512], scalar=c.bsT[:, g:g + 1],
                        in1=ug[:, lo - s * 512:hi - s * 512], op0=ALU.add, op1=ALU.mult),
                      r=[bpg, c.b_bsT, b_pT[ui]], w=[bcc] if n_ == 0 else (), cw=() if n_ == 0 else [bcc])
                cat_chunk_to_catT(cc, bcc, 4 * s, t)
                if s == 2:
                    mem_attn_tile(t)
        return a_w_out[i]

    def outproj_ln_route(l, w_out, x_src):
        sls = [load_slab_in(s, w_out, s * 512) for s in range(4)]
        bcast_load(gB[:], ln_g[l - L0, 0], b_gB, D)
        E("sp", lambda e: e.dma_start(out=bB[:], in_=ln_b[l - L0, 0].partition_broadcast(128)), cw=[b_gB], dma=b_gB)
        E("sp", lambda e: e.dma_start(out=wr[:], in_=moe_wr[l - L0].rearrange("(k p) n -> p k n", p=128)), w=[b_wr], dma=b_wr)
        E("sp", lambda e: e.dma_start(out=brB[:], in_=moe_br[l - L0].partition_broadcast(128)), cw=[b_wr], dma=b_wr)
        for t in range(T):
            i = t % 2
            xt, bx = xw[i], b_xw[i]
            E("sp", lambda e, xt=xt, t=t: e.dma_start(out=xt[:], in_=x_src[t * 128:(t + 1) * 128, :]),
              r=[b_x_d[t]] if x_src is not x_in else (), w=[bx], dma=bx)
            for s in range(4):
                ps, bp = c.next_ps()
                for k in range(16):
                    E("pe", lambda e, ps=ps, k=k, s=s, t=t: e.matmul(ps[:], lhsT=catT[:, k, t * 128:(t + 1) * 128], rhs=sls[s][:, k, :],
                                                                    start=(k == 0), stop=(k == 15)),
                      r=[bw[s], b_catT[t]] + CG, w=[bp] if k == 0 else (), cw=() if k == 0 else [bp])
                E("dve", lambda e, ps=ps, s=s, xt=xt: e.scalar_tensor_tensor(out=xt[:, s * 512:(s + 1) * 512], in0=xt[:, s * 512:(s + 1) * 512],
                                                                          scalar=ALPHA, in1=ps[:], op0=ALU.mult, op1=ALU.add),
                  r=[bp], w=[bx])
            ln_tile(xt[:], bx, t, D, gB[:], bB[:], b_gB)
            E("sp", lambda e, xt=xt, t=t: e.dma_start(out=xm_d[t * 128:(t + 1) * 128, :], in_=xt[:]), r=[bx], w=[b_xm_d[t]], dma=b_xm_d[t])
            route_tile(l, t, xt, bx)

    def route_tile(l, t, xt, bx):
        fi = t % 2
        xf, bxf = xTf[fi], b_xTf[fi]
        transpose_tile_f32(xt, bx, lambda g: xf[:, 4 * g:4 * g + 4, :], [bxf])
        ps, bp = c.next_ps()
        for k in range(16):
            E("pe", lambda e, k=k: e.matmul(ps[:, 0:36], lhsT=xf[:, k, :], rhs=wr[:, k, :], start=(k == 0), stop=(k == 15)),
              r=[bxf, b_wr], w=[bp] if k == 0 else (), cw=() if k == 0 else [bp])
        R = rt[:, t, :]
        br_ = b_rt[t]
        lg = R[:, 0:36]
        V = lambda e: e
        D_ = lambda fn, r=(), first=False: E("dve", fn, r=list(r) + [br_], w=[br_]) if not first else E("dve", fn, r=list(r), w=[br_])
        D_(lambda e: e.tensor_tensor(out=lg, in0=ps[:, 0:36], in1=brB[:], op=ALU.add), r=[bp, b_wr], first=True)
        m1 = R[:, 36:37]
        D_(lambda e: e.tensor_reduce(out=m1, in_=R[:, 0:4], axis=AX.X, op=ALU.max))
        oh1 = R[:, 40:44]
        D_(lambda e: e.tensor_scalar(out=oh1, in0=R[:, 0:4], scalar1=m1, scalar2=None, op0=ALU.is_equal))
        nm1 = R[:, 37:38]
        D_(lambda e: e.tensor_scalar(out=nm1, in0=m1, scalar1=-1.0, scalar2=None, op0=ALU.mult))
        e1 = R[:, 44:48]
        s1 = R[:, 38:39]
        E("act", lambda e: e.activation(out=e1, in_=R[:, 0:4], func=AF.Exp, bias=nm1, scale=1.0, accum_out=s1), r=[br_], w=[br_])
        psel = R[:, 39:40]
        D_(lambda e: e.reciprocal(out=psel, in_=s1))
        pen = R[:, 48:52]
        D_(lambda e: e.tensor_scalar(out=pen, in0=oh1, scalar1=1.0e9, scalar2=-1.0e9, op0=ALU.mult, op1=ALU.add))
        lg2 = R[:, 52:84]
        D_(lambda e: e.tensor_tensor(out=lg2.rearrange("p (g k) -> p g k", g=4), in0=R[:, 4:36].rearrange("p (g k) -> p g k", g=4),
                                     in1=pen.unsqueeze(2).to_broadcast([128, 4, 8]), op=ALU.add))
        top8 = R[:, 84:92]
        D_(lambda e: e.max(out=top8, in_=lg2))
        ohA = R[:, 92:124]
        D_(lambda e: e.tensor_scalar(out=ohA, in0=lg2, scalar1=top8[:, 0:1], scalar2=None, op0=ALU.is_equal))
        ohB = R[:, 124:156]
        D_(lambda e: e.tensor_scalar(out=ohB, in0=lg2, scalar1=top8[:, 1:2], scalar2=None, op0=ALU.is_equal))
        dd = R[:, 156:157]
        D_(lambda e: e.tensor_tensor(out=dd, in0=top8[:, 1:2], in1=top8[:, 0:1], op=ALU.subtract))
        ed = R[:, 157:158]
        E("act", lambda e: e.activation(out=ed, in_=dd, func=AF.Exp), r=[br_], w=[br_])
        D_(lambda e: e.tensor_scalar(out=ed, in0=ed, scalar1=1.0, scalar2=None, op0=ALU.add))
        sg = R[:, 158:159]
        D_(lambda e: e.reciprocal(out=sg, in_=ed))
        gA = gates[:, t, 0:1]
        gBt = gates[:, t, 1:2]
        E("dve", lambda e: e.tensor_tensor(out=gA, in0=sg, in1=psel, op=ALU.mult), r=[br_], w=[b_gs[t]])
        E("dve", lambda e: e.tensor_tensor(out=gBt, in0=psel, in1=gA, op=ALU.subtract), r=[br_], w=[b_gs[t]])
        E("dve", lambda e: e.tensor_tensor(out=Ab[:, t, :], in0=ohA, in1=ohB, op=ALU.add), r=[br_], w=[b_Ab[t]])
        psr, bpr = c.next_ps()
        E("pe", lambda e: e.matmul(psr[:, 0:32], lhsT=ustrb[:], rhs=Ab[:, t, :], start=True, stop=(t == 0)),
          r=[b_cb, b_Ab[t]], w=[bpr])
        for t2 in range(t):
            E("pe", lambda e, t2=t2: e.matmul(psr[:, 0:32], lhsT=onesb[:], rhs=Ab[:, t2, :], start=False, stop=(t2 == t - 1)),
              r=[b_cb, b_Ab[t2]], cw=[bpr])
        rk = R[:, 0:32]
        D_(lambda e: e.tensor_copy(out=rk, in_=psr[:, 0:32]), r=[bpr])
        for n_, (oh, gcol) in enumerate(((ohA, gA), (ohB, gBt))):
            sel = R[:, 36 + n_:37 + n_]
            prod = R[:, 52:84]
            D_(lambda e, oh=oh: e.tensor_tensor(out=prod, in0=oh, in1=rk, op=ALU.mult))
            D_(lambda e, sel=sel: e.tensor_reduce(out=sel, in_=prod, axis=AX.X, op=ALU.add))
            D_(lambda e, oh=oh: e.tensor_tensor(out=prod, in0=oh, in1=ebase, op=ALU.mult), r=[b_cst])
            base = R[:, 38 + n_:39 + n_]
            D_(lambda e, base=base: e.tensor_reduce(out=base, in_=prod, axis=AX.X, op=ALU.add))
            ok = R[:, 44 + n_:45 + n_]
            D_(lambda e, ok=ok, sel=sel: e.tensor_scalar(out=ok, in0=sel, scalar1=float(CAP) - 0.5, scalar2=None, op0=ALU.is_lt))
            sf = R[:, 46 + n_:47 + n_]
            D_(lambda e, sf=sf, base=base, sel=sel: e.tensor_tensor(out=sf, in0=base, in1=sel, op=ALU.add))
            D_(lambda e, sf=sf: e.tensor_tensor(out=sf, in0=sf, in1=trashc, op=ALU.subtract), r=[b_cst])
            D_(lambda e, sf=sf, ok=ok: e.tensor_tensor(out=sf, in0=sf, in1=ok, op=ALU.mult))
            D_(lambda e, sf=sf: e.tensor_tensor(out=sf, in0=sf, in1=trashc, op=ALU.add), r=[b_cst])
            E("dve", lambda e, sf=sf, n_=n_: e.tensor_copy(out=slots[:, t, n_:n_ + 1], in_=sf), r=[br_], w=[b_gs[t]] if False else (), cw=[b_gs[t]])
            E("dve", lambda e, gcol=gcol, ok=ok: e.tensor_tensor(out=gcol, in0=gcol, in1=ok, op=ALU.mult), r=[br_], cw=[b_gs[t]])
        xb, bxb = xbf[fi], b_xbf[fi]
        E("act", lambda e: e.activation(out=xb[:], in_=xt[:], func=AF.Copy), r=[bx], w=[bxb])
        for n_ in range(2):
            E("pool", lambda e, n_=n_: e.indirect_dma_start(out=xbuf, out_offset=bass.IndirectOffsetOnAxis(ap=slots[:, t, n_:n_ + 1], axis=0),
                                                            in_=xb[:], in_offset=None),
              r=[bxb, b_gs[t]], cw=[b_xbuf], dma=b_xbuf)

    def experts(l):
        for ex in range(32):
            s1, s3, s2 = [(3 * ex + j) % 6 for j in range(3)]
            w1 = load_slab_in(s1, moe_w1[l - L0, ex], 0)
            w3 = load_slab_in(s3, moe_w3[l - L0, ex], 0)
            w2v = slab_w2(s2)
            E("pool", lambda e, w2v=w2v, ex=ex: e.dma_start(out=w2v, in_=moe_w2[l - L0, ex].rearrange("(k p) n -> p k n", p=128)),
              w=[bw[s2]], dma=bw[s2])
            i = ex % 2
            E("sp", lambda e, i=i, ex=ex: e.dma_start(out=xe[i], in_=xbuf[ex * CAP:(ex + 1) * CAP, :]), r=[b_xbuf], w=[b_xe[i]], dma=b_xe[i])
            for g in range(2):
                ps, bp = c.next_ps()
                psb = ps[:].bitcast(BF16)
                for j in range(8):
                    k = 8 * g + j
                    E("pe", lambda e, psb=psb, j=j, k=k, i=i: e.transpose(out=psb[:, j * 128:(j + 1) * 128], in_=xe[i][:, k * 128:(k + 1) * 128],
                                                                        identity=identb[:]),
                      r=[b_xe[i], b_cb], w=[bp] if j == 0 else (), cw=() if j == 0 else [bp])
                c.evac(xeT[i][:, 8 * g:8 * g + 8, :], psb.rearrange("p (a b) -> p a b", a=8), r=[bp],
                       w=[b_xeT[i]] if g == 0 else (), cw=() if g == 0 else [b_xeT[i]])
            ps1, bp1 = c.next_ps()
            ps3, bp3 = c.next_ps()
            for (ps, bp, wv, sb_) in ((ps1, bp1, w1, s1), (ps3, bp3, w3, s3)):
                for k in range(16):
                    E("pe", lambda e, ps=ps, k=k, wv=wv, i=i: e.matmul(ps[:], lhsT=xeT[i][:, k, :], rhs=wv[:, k, :], start=(k == 0), stop=(k == 15)),
                      r=[b_xeT[i], bw[sb_]], w=[bp] if k == 0 else (), cw=() if k == 0 else [bp])
            E("act", lambda e, i=i: e.activation(out=hs[i], in_=ps1[:], func=AF.Silu), r=[bp1], w=[b_hs[i]])
            E("dve", lambda e, i=i: e.tensor_tensor(out=hb[i], in0=hs[i], in1=ps3[:], op=ALU.mult), r=[b_hs[i], bp3], w=[b_hb[i]])
            ps, bp = c.next_ps()
            psb = ps[:].bitcast(BF16)
            for j in range(4):
                E("pe", lambda e, psb=psb, j=j, i=i: e.transpose(out=psb[:, j * 128:(j + 1) * 128], in_=hb[i][:, j * 128:(j + 1) * 128], identity=identb[:]),
                  r=[b_hb[i], b_cb], w=[bp] if j == 0 else (), cw=() if j == 0 else [bp])
            c.evac(hT[i], psb[:, 0:512].rearrange("p (a b) -> p a b", a=4), r=[bp], w=[b_hT[i]])
            for n4 in range(4):
                ps, bp = c.next_ps()
                for k in range(4):
                    E("pe", lambda e, ps=ps, k=k, n4=n4, i=i: e.matmul(ps[:], lhsT=hT[i][:, k, :], rhs=w2v[:, k, n4 * 512:(n4 + 1) * 512],
                                                                      start=(k == 0), stop=(k == 3)),
                      r=[b_hT[i], bw[s2]], w=[bp] if k == 0 else (), cw=() if k == 0 else [bp])
                c.evac(ye[i][:, n4 * 512:(n4 + 1) * 512], ps[:], r=[bp], w=[b_ye[i]] if n4 == 0 else (), cw=() if n4 == 0 else [b_ye[i]])
            E("sp", lambda e, i=i, ex=ex: e.dma_start(out=ybuf[ex * CAP:(ex + 1) * CAP, :], in_=ye[i]), r=[b_ye[i]], w=[b_ybuf[ex]], dma=b_ybuf[ex])

    def combine(l, last, need_xT):
        bcast_load(gB[:], ln_g[l - L0, 1], b_gB, D)
        E("sp", lambda e: e.dma_start(out=bB[:], in_=ln_b[l - L0, 1].partition_broadcast(128)), cw=[b_gB], dma=b_gB)
        for t in range(T):
            i = t % 2
            xt, bx = xw[i], b_xw[i]
            E("sp", lambda e, xt=xt, t=t: e.dma_start(out=xt[:], in_=xm_d[t * 128:(t + 1) * 128, :]), r=[b_xm_d[t]], w=[bx], dma=bx)
            for n_ in range(2):
                E("pool", lambda e, i=i, n_=n_, t=t: e.indirect_dma_start(out=y01[i][:, n_, :], out_offset=None, in_=ybuf,
                                                                         in_offset=bass.IndirectOffsetOnAxis(ap=slots[:, t, n_:n_ + 1], axis=0)),
                  r=b_ybuf + [b_gs[t]], w=[b_y01[i]] if n_ == 0 else (), cw=() if n_ == 0 else [b_y01[i]], dma=b_y01[i])
            E("dve", lambda e, xt=xt: e.tensor_scalar(out=xt[:], in0=xt[:], scalar1=ALPHA, scalar2=None, op0=ALU.mult), w=[bx])
            for n_ in range(2):
                E("dve", lambda e, xt=xt, i=i, n_=n_, t=t: e.scalar_tensor_tensor(out=xt[:], in0=y01[i][:, n_, :], scalar=gates[:, t, n_:n_ + 1],
                                                                                 in1=xt[:], op0=ALU.mult, op1=ALU.add),
                  r=[b_y01[i], b_gs[t]], w=[bx])
            ln_tile(xt[:], bx, t, D, gB[:], bB[:], b_gB)
            if last:
                E("sp", lambda e, xt=xt, t=t: e.dma_start(out=y_out[t * 128:(t + 1) * 128, :], in_=xt[:]), r=[bx], w=[b_y[t]], dma=b_y[t])
            else:
                E("sp", lambda e, xt=xt, t=t: e.dma_start(out=x_d[t * 128:(t + 1) * 128, :], in_=xt[:]), r=[bx], w=[b_x_d[t]], dma=b_x_d[t])
            if need_xT:
                make_xT(xt, bx, t)

    def kv_project():
        Wkv = shared_w_kv
        ktb = catc[0:2]
        b_ktb = b_catc[0:2]
        vtb = c.vreg[:, 0:T * 6 * 257].rearrange("p (t h e) -> p t h e", t=T, h=6)
        for t in range(T):
            E("dve", lambda e, t=t: e.memset(vtb[:, t, :, 256:257], 1.0), w=[c.b_v[t]])
        n = 0
        for s in range(3):
            sl = load_slab_in(s % 2, Wkv, s * 512)
            for fc4 in range(4):
                fc = 4 * s + fc4
                for half in range(2):
                    ps, bp = c.next_ps()
                    for k in range(16):
                        E("pe", lambda e, ps=ps, k=k, fc4=fc4, half=half, sl=sl: e.matmul(
                            ps[:], lhsT=sl[:, k, fc4 * 128:(fc4 + 1) * 128], rhs=xT[:, k, half * 512:(half + 1) * 512],
                            start=(k == 0), stop=(k == 15)),
                          r=[bw[s % 2]] + b_xT[4 * half:4 * half + 4] + XG, w=[bp] if k == 0 else (), cw=() if k == 0 else [bp])
                    i = n % 2
                    n += 1
                    c.evac(ktb[i][:], ps[:], r=[bp], w=[b_ktb[i]])
                    E("sp", lambda e, i=i, fc=fc, half=half: e.dma_start(
                        out=kt_out[4 * half:4 * half + 4, fc // 2, :, fc % 2, :].rearrange("t d k -> d t k"),
                        in_=ktb[i][:].rearrange("p (t k) -> p t k", t=4)), r=[b_ktb[i]], cw=[c.b_kt], dma=c.b_kt)
        for s in range(3):
            sl = load_slab_in(s % 2, Wkv, 1536 + s * 512)
            for t in range(T):
                ps, bp = c.next_ps()
                for k in range(16):
                    E("pe", lambda e, ps=ps, k=k, t=t, sl=sl: e.matmul(ps[:], lhsT=xT[:, k, t * 128:(t + 1) * 128], rhs=sl[:, k, :],
                                                                      start=(k == 0), stop=(k == 15)),
                      r=[bw[s % 2], b_xT[t]] + XG, w=[bp] if k == 0 else (), cw=() if k == 0 else [bp])
                c.evac(vtb[:, t, 2 * s:2 * s + 2, 0:256], ps[:].rearrange("p (h e) -> p h e", h=2), r=[bp], cw=[c.b_v[t]])
        for t in range(T):
            if fused:
                E("sp", lambda e, t=t: e.dma_start(out=vl_v[t].rearrange("h k e -> k h e"), in_=vtb[:, t, :, :]), r=[c.b_v[t]],
                  w=[c.b_vout[t]], dma=c.b_vout[t])
            else:
                E("sp", lambda e, t=t: e.dma_start(out=v_out[t], in_=vtb[:, t, :, :]), r=[c.b_v[t]], w=[c.b_vout[t]], dma=c.b_vout[t])


    def build_tables():
        rb = xw[0][:, 0:192].rearrange("p (b h) -> p b h", b=32)
        E("sp", lambda e: e.dma_start(out=xw[0][:, 0:192], in_=rel_bias.rearrange("b h -> (b h)").partition_broadcast(128)),
          w=[b_xw[0]], dma=b_xw[0])
        rbs = xw[0][:, 256:448].rearrange("p (b h) -> p b h", b=32)
        E("dve", lambda e: e.tensor_tensor(out=rbs, in0=rb, in1=rb[:, 15:16, :].to_broadcast([128, 32, 6]), op=ALU.subtract), w=[b_xw[0]])
        E("dve", lambda e: e.tensor_scalar(out=rbs, in0=rbs, scalar1=1.0 / SCALE, scalar2=None, op0=ALU.mult), w=[b_xw[0]])
        bi = xw[1][:, 0:256]
        E("sp", lambda e: e.dma_start(out=bi, in_=bidx.rearrange("p a q -> p (a q)")), w=[b_xw[1]], dma=b_xw[1])
        acc = xw[1][:, 256:512]
        tmp = xw[1][:, 512:768]
        for h in range(6):
            E("dve", lambda e: e.tensor_scalar(out=acc, in0=bi, scalar1=32.0, scalar2=NEG, op0=ALU.is_equal, op1=ALU.mult), w=[b_xw[1]])
            for bk in range(32):
                E("dve", lambda e, bk=bk, h=h: e.tensor_scalar(out=tmp, in0=bi, scalar1=float(bk), scalar2=rbs[:, bk, h:h + 1],
                                                              op0=ALU.is_equal, op1=ALU.mult), r=[b_xw[0]], w=[b_xw[1]])
                E("dve", lambda e: e.tensor_tensor(out=acc, in0=acc, in1=tmp, op=ALU.add), w=[b_xw[1]])
            E("dve", lambda e, h=h: e.tensor_copy(out=c.tbl[:, h, :, :], in_=acc.rearrange("p (a q) -> p a q", a=2)), r=[b_xw[1]],
              w=[c.b_tbl] if h == 0 else (), cw=() if h == 0 else [c.b_tbl])
        E("sp", lambda e: e.dma_start(out=c.fmask[:], in_=farmask), w=[c.b_fmask], dma=c.b_fmask)

    def lam_params(i, l):
        lam_init = 0.8 - 0.6 * math.exp(-0.3 * l)
        lp = xw[0][:, 0:512]
        E("sp", lambda e: e.dma_start(out=lp, in_=b_lambda[i].rearrange("a d -> (a d)").partition_broadcast(128)), w=[b_xw[0]], dma=b_xw[0])
        pr = xw[0][:, 512:768]
        sc = c.lamt
        E("dve", lambda e: e.tensor_tensor(out=pr.rearrange("p (a d) -> p a d", a=2), in0=lp.rearrange("p (a d) -> p a d", a=4)[:, 0:4:2, :],
                                           in1=lp.rearrange("p (a d) -> p a d", a=4)[:, 1:4:2, :], op=ALU.mult), w=[b_xw[0]])
        E("dve", lambda e: e.tensor_reduce(out=sc[:, 0:2], in_=pr.rearrange("p (a d) -> p a d", a=2), axis=AX.X, op=ALU.add), r=[b_xw[0]], w=[c.b_lamt])
        E("act", lambda e: e.activation(out=sc[:, 2:4], in_=sc[:, 0:2], func=AF.Exp), w=[c.b_lamt])
        E("dve", lambda e: e.tensor_tensor(out=sc[:, 4:5], in0=sc[:, 2:3], in1=sc[:, 3:4], op=ALU.subtract), w=[c.b_lamt])
        E("dve", lambda e: e.tensor_scalar(out=sc[:, 5:6], in0=sc[:, 4:5], scalar1=lam_init, scalar2=-1.0, op0=ALU.add, op1=ALU.mult), w=[c.b_lamt])
        E("sp", lambda e: e.dma_start(out=c.gsub[:], in_=b_subln_g[i].partition_broadcast(128)), w=[c.b_gsub], dma=c.b_gsub)
        E("dve", lambda e: e.tensor_scalar(out=c.gsub[:], in0=c.gsub[:], scalar1=1.0 - lam_init, scalar2=None, op0=ALU.mult), w=[c.b_gsub])

    def attn_tile_head(t, h, qh, b_qh):
        nfar = LT[t] - 2
        chunks = [("near", 0, 2)] + [("far", j0, min(4, nfar - j0)) for j0 in range(0, nfar, 4)]
        oi = c.oi
        c.oi = (oi + 1) % 2
        O = [c.ps[(0, 6)[oi]], c.ps[(1, 7)[oi]]]
        bO = [c.bps[(0, 6)[oi]], c.bps[(1, 7)[oi]]]
        nsteps = 2 + nfar
        state = {"first_pv": True, "done": 0}

        def emit_pv(pr):
            (ks, vch, p0, pis) = pr
            for si in range(2):
                jj = p0 + si
                pt_ = c.ptring[pis[si]]
                state["done"] += 1
                last = (state["done"] == nsteps)
                fp = state["first_pv"]
                for m in range(2):
                    E("pe", lambda e, m=m, pt_=pt_, jj=jj, fp=fp, last=last: e.matmul(O[m][:, 0:257], lhsT=pt_[:, m * 128:(m + 1) * 128],
                                                                                     rhs=vch[:, jj, :], start=fp, stop=last),
                      r=[c.b_ptring[pis[si]], c.b_vring[ks]], w=[bO[m]] if fp else (), cw=() if fp else [bO[m]])
                state["first_pv"] = False

        pending = []
        for (ckind, j0, nb) in chunks:
            ks = c.kvi
            c.kvi = (ks + 1) % c.NKV
            kch, vch = c.kring[ks], c.vring[ks]
            if ckind == "near":
                ksrc = near_k[t, h]
                vsrc = near_v[t, h]
            else:
                ksrc = g_k[h, :, j0:j0 + nb]
                vsrc = g_v[h, :, j0:j0 + nb, :]
            E("sp", lambda e: e.dma_start(out=kch[:, 0:nb], in_=ksrc), w=[c.b_kring[ks]], dma=c.b_kring[ks])
            E("sp", lambda e: e.dma_start(out=vch[:, 0:nb, :], in_=vsrc), w=[c.b_vring[ks]], dma=c.b_vring[ks])
            for p0 in range(0, nb, 2):
                bank = 2 + c.sbank
                c.sbank = (c.sbank + 1) % 4
                ps, bp = c.ps[bank], c.bps[bank]
                for si in range(2):
                    jj = p0 + si
                    for m in range(2):
                        col = (2 * si + m) * 128
                        first = (si == 0 and m == 0)
                        E("pe", lambda e, col=col, jj=jj, m=m: e.matmul(ps[:, col:col + 128], lhsT=kch[:, jj, m, :],
                                                                        rhs=qh[:, m, t * 128:(t + 1) * 128],
                                                                        start=True, stop=(ckind != "near")),
                          r=[c.b_kring[ks], b_qh], w=[bp] if first else (), cw=() if first else [bp])
                        if ckind == "near":
                            E("pe", lambda e, col=col, jj=jj: e.matmul(ps[:, col:col + 128], lhsT=identb[:], rhs=c.tbl[:, h, jj, :],
                                                                     start=False, stop=True),
                              r=[b_cb, c.b_tbl], cw=[bp])
                pis = []
                for si in range(2):
                    jj = p0 + si
                    pi = c.pti
                    c.pti = (pi + 1) % 8
                    pis.append(pi)
                    pt_ = c.ptring[pi]
                    if ckind == "near":
                        E("act", lambda e, si=si, pt_=pt_: e.activation(out=pt_, in_=ps[:, si * 256:(si + 1) * 256], func=AF.Exp, scale=SCALE),
                          r=[bp], w=[c.b_ptring[pi]])
                    else:
                        E("act", lambda e, si=si, pt_=pt_, jj=jj: e.activation(out=pt_, in_=ps[:, si * 256:(si + 1) * 256], func=AF.Exp,
                                                                            scale=SCALE, bias=c.fmask[:, t, j0 + jj:j0 + jj + 1]),
                          r=[bp, c.b_fmask], w=[c.b_ptring[pi]])
                pending.append((ks, vch, p0, pis))
                if len(pending) > 1:
                    emit_pv(pending.pop(0))
        while pending:
            emit_pv(pending.pop(0))
        sc = c.asc[:, c.asi * 8:(c.asi + 1) * 8]
        bsc = c.b_asc[c.asi]
        c.asi = (c.asi + 1) % 4
        E("dve", lambda e: e.reciprocal(out=sc[:, 0:1], in_=O[0][:, 256:257]), r=[bO[0]], w=[bsc])
        E("dve", lambda e: e.reciprocal(out=sc[:, 1:2], in_=O[1][:, 256:257]), r=[bO[1]], w=[bsc])
        E("dve", lambda e: e.tensor_tensor(out=sc[:, 2:3], in0=sc[:, 1:2], in1=c.lamt[:, 5:6], op=ALU.mult), r=[c.b_lamt], w=[bsc])
        oi_ = c.ofi
        c.ofi = (oi_ + 1) % 2
        of, bof = c.of[oi_], c.b_of[oi_]
        E("dve", lambda e: e.tensor_scalar(out=of[:, 256:512], in0=O[1][:, 0:256], scalar1=sc[:, 2:3], scalar2=None, op0=ALU.mult),
          r=[bO[1], bsc], w=[bof])
        E("dve", lambda e: e.scalar_tensor_tensor(out=of[:, 0:256], in0=O[0][:, 0:256], scalar=sc[:, 0:1], in1=of[:, 256:512],
                                                 op0=ALU.mult, op1=ALU.add), r=[bO[0], bsc], w=[bof])
        E("act", lambda e: e.activation(out=of[:, 256:512], in_=of[:, 0:256], func=AF.Square, accum_out=sc[:, 3:4]), r=[bsc], w=[bof])
        E("dve", lambda e: e.tensor_scalar(out=sc[:, 4:5], in0=sc[:, 3:4], scalar1=1.0 / 256.0, scalar2=EPS, op0=ALU.mult, op1=ALU.add),
          r=[bof], w=[bsc])
        E("act", lambda e: e.activation(out=sc[:, 5:6], in_=sc[:, 4:5], func=AF.Ln), w=[bsc])
        E("act", lambda e: e.activation(out=sc[:, 6:7], in_=sc[:, 5:6], func=AF.Exp, scale=-0.5), w=[bsc])
        ci = rot("catc", 3)
        cc, bcc = catc[ci], b_catc[ci]
        E("dve", lambda e: e.scalar_tensor_tensor(out=cc[:, 0:256], in0=of[:, 0:256], scalar=sc[:, 6:7], in1=c.gsub[:], op0=ALU.mult, op1=ALU.mult),
          r=[bof, bsc, c.b_gsub], w=[bcc])
        cat_chunk_to_catT(cc, bcc, 2 * h, t, n=2)

    def mixer_B(l):
        i = l - 2
        Wq = b_w_in[i]
        lam_params(i, l)
        sl = load_slab_in(1, Wq, 1536)
        q_featmajor(sl, bw[1], 4, lambda fc, half: qmT[:, fc, half * 512:(half + 1) * 512], b_qmT)
        mem_kv(l)
        for h in range(6):
            si = h % 2
            sl = load_slab_in(si, Wq, h * 256, ncols=256)
            qh = c.qh[h % 2]
            bqh = c.b_qh[h % 2]
            q_featmajor(sl, bw[si], 2, lambda fc, half, qh=qh: qh[:, fc, half * 512:(half + 1) * 512], bqh)
            for t in range(T):
                attn_tile_head(t, h, qh, bqh)
                if h == 2:
                    mem_attn_tile(t)
        return b_w_out[i]

    c.b_v = P.bufs(T, "vv")
    c.wsT = c.sb("wsT", [128, 4, 128], BF16)
    c.b_wsT = P.buf("wsT")
    c.bsT = c.sb("bsT", [128, 4], F32)
    c.b_bsT = P.buf("bsT")
    c.b_kt = P.buf("ktout")
    c.b_vout = P.bufs(T, "vout")
    P.share(c.b_vout)
    if mode in ("B", "F"):
        vr2 = c.vreg
        c.tbl = vr2[:, 0:1536].rearrange("p (h a q) -> p h a q", h=6, a=2)
        c.b_tbl = P.buf("tbl")
        c.NKV = 3
        o_k = 1536
        o_v = o_k + c.NKV * 1024
        o_p = o_v + c.NKV * 1028
        o_q = o_p + 8 * 256
        o_f = o_q + 2 * 2048
        assert o_f + 2048 <= 16896
        c.kring = [vr2[:, o_k + i * 1024:o_k + (i + 1) * 1024].rearrange("p (j m k) -> p j m k", j=4, m=2) for i in range(c.NKV)]
        c.vring = [vr2[:, o_v + i * 1028:o_v + (i + 1) * 1028].rearrange("p (j e) -> p j e", j=4) for i in range(c.NKV)]
        c.b_kring = P.bufs(c.NKV, "kr")
        c.b_vring = P.bufs(c.NKV, "vr")
        c.ptring = [vr2[:, o_p + i * 256:o_p + (i + 1) * 256] for i in range(8)]
        c.b_ptring = P.bufs(8, "ptr")
        c.qh = [vr2[:, o_q + i * 2048:o_q + (i + 1) * 2048].rearrange("p (m n) -> p m n", m=2) for i in range(2)]
        c.b_qh = P.bufs(2, "qh")
        c.of = [vr2[:, o_f + i * 1024:o_f + (i + 1) * 1024].bitcast(F32) for i in range(2)]
        c.b_of = P.bufs(2, "of")
        c.fmask = c.sb("fmask", [128, T, 64], F32)
        c.b_fmask = P.buf("fmask")
        c.lamt = c.sb("lamt", [128, 8], F32)
        c.b_lamt = P.buf("lamt")
        c.gsub = c.sb("gsub", [128, 256], F32)
        c.b_gsub = P.buf("gsub")
        c.asc = c.sb("asc", [128, 32], F32)
        c.b_asc = P.bufs(4, "asc")
        c.oi = c.sbank = c.kvi = c.pti = c.asi = c.ofi = 0
        if fused:
            c.pidx = c.sb("pidx", [128, T, 6], U32)
            c.nmask = c.sb("nmask", [128, T], F32)
            c.b_pidx = P.buf("pidx")
            c.b_gk = P.buf("gk")
            E("sp", lambda e: e.dma_start(out=c.pidx[:], in_=previdx), w=[c.b_pidx], dma=c.b_pidx)
            E("sp", lambda e: e.dma_start(out=c.nmask[:], in_=nearmask), cw=[c.b_pidx], dma=c.b_pidx)

    if mode == "F":
        for l in (0, 1):
            w_out = mixer_A(l)
            outproj_ln_route(l, w_out, x_in if l == 0 else x_d)
            experts(l)
            combine(l, last=False, need_xT=True)
        kv_project()
        rg = [list(range(NCORES))]
        E("pool", lambda e: e.collective_compute("AllGather", ALU.bypass, rg, ins=[ktl2], outs=[gk2]), r=[c.b_kt], w=[c.b_gk], dma=c.b_gk)
        E("pool", lambda e: e.collective_compute("AllGather", ALU.bypass, rg, ins=[vl2], outs=[gv2]), r=c.b_vout, cw=[c.b_gk], dma=c.b_gk)
        P.fence([c.b_gk])
        for l in (2, 3):
            build_tables()
            w_out = mixer_B(l)
            outproj_ln_route(l, w_out, x_d)
            experts(l)
            combine(l, last=(l == 3), need_xT=(l == 2))
        P.final_wait("sp", b_y)
    elif mode == "A":
        for l in layers:
            w_out = mixer_A(l)
            if debug and l == layers[-1]:
                b_dbg2 = P.bufs(2, "dbg2")
                E("sp", lambda e: e.dma_start(out=dbg_catT, in_=catT), r=b_catT + CG, w=[b_dbg2[0]], dma=b_dbg2[0])
                E("sp", lambda e: e.dma_start(out=dbg_vv, in_=c.vreg[:, 0:T * 1536]), r=c.b_v, w=[b_dbg2[1]], dma=b_dbg2[1])
            outproj_ln_route(l, w_out, x_in if l == 0 else x_d)
            experts(l)
            combine(l, last=False, need_xT=True)
            if debug and l == layers[-1]:
                b_dbg = P.bufs(5, "dbg")
                E("sp", lambda e: e.dma_start(out=dbg_slots, in_=slots[:]), r=b_gs, w=[b_dbg[0]], dma=b_dbg[0])
                E("sp", lambda e: e.dma_start(out=dbg_gates, in_=gates[:]), r=b_gs, w=[b_dbg[1]], dma=b_dbg[1])
                E("sp", lambda e: e.dma_start(out=dbg_xbuf, in_=xbuf), r=[b_xbuf], w=[b_dbg[2]], dma=b_dbg[2])
                E("sp", lambda e: e.dma_start(out=dbg_ybuf, in_=ybuf), r=b_ybuf, w=[b_dbg[3]], dma=b_dbg[3])
                E("sp", lambda e: e.dma_start(out=dbg_xm, in_=xm_d), r=b_xm_d, w=[b_dbg[4]], dma=b_dbg[4])
                P.final_wait("sp", b_dbg + b_dbg2)
        kv_project()
        for t in range(T):
            i = t % 2
            E("sp", lambda e, i=i, t=t: e.dma_start(out=xw[i][:], in_=x_d[t * 128:(t + 1) * 128, :]), r=[b_x_d[t]], w=[b_xw[i]], dma=b_xw[i])
            E("sp", lambda e, i=i, t=t: e.dma_start(out=y_out[t * 128:(t + 1) * 128, :], in_=xw[i][:]), r=[b_xw[i]], w=[b_y[t]], dma=b_y[t])
        P.final_wait("sp", b_y + [c.b_kt] + c.b_vout)
    else:
        tbl_d = c.dram("tbl_d", [128, 1536], BF16)
        b_tbl_d = P.buf("tbl_d")
        for l in layers:
            if l == layers[0]:
                build_tables()
                E("sp", lambda e: e.dma_start(out=tbl_d, in_=c.vreg[:, 0:1536]), r=[c.b_tbl], w=[b_tbl_d], dma=b_tbl_d)
            else:
                E("sp", lambda e: e.dma_start(out=c.vreg[:, 0:1536], in_=tbl_d), r=[b_tbl_d], w=[c.b_tbl, b_xe[0]], dma=c.b_tbl)
                E("sp", lambda e: e.dma_start(out=c.fmask[:], in_=farmask), w=[c.b_fmask], dma=c.b_fmask)
            w_out = mixer_B(l)
            if debug and l == 2:
                b_dbg3 = P.bufs(4, "dbg3")
                E("sp", lambda e: e.dma_start(out=dbg_catT, in_=catT), r=b_catT + CG, w=[b_dbg3[0]], dma=b_dbg3[0])
                E("sp", lambda e: e.dma_start(out=dbg_tbl, in_=c.vreg[:, 0:1536]), r=[c.b_tbl], w=[b_dbg3[1]], dma=b_dbg3[1])
                E("sp", lambda e: e.dma_start(out=dbg_qh, in_=c.vreg[:, 7184 + 2048:7184 + 4096]), r=c.b_qh, w=[b_dbg3[2]], dma=b_dbg3[2])
                E("sp", lambda e: e.dma_start(out=dbg_lam, in_=c.lamt[:]), r=[c.b_lamt], w=[b_dbg3[3]], dma=b_dbg3[3])
                P.final_wait("sp", b_dbg3)
            outproj_ln_route(l, w_out, x_in if l == 2 else x_d)
            experts(l)
            combine(l, last=(l == layers[-1]), need_xT=(l == 2 and len(layers) > 1))
        P.final_wait("sp", b_y)
    stats = P.finalize()
    st.close()
    return nc, stats


def make_consts(core):
    cst = np.zeros((128, 448), np.float32)
    cst[:, 0:128] = np.eye(128, dtype=np.float32)
    p = np.arange(128)
    cst[:, 128:256] = (p[:, None] < p[None, :]).astype(np.float32)
    cst[:, 256:384] = ((p[None, :] // 64) <= (p[:, None] // 64)).astype(np.float32)
    cst[:, 384:416] = (np.arange(32) * CAP)[None, :].astype(np.float32)
    cst[:, 416] = NSLOT + p
    return cst


def shard_tokens(x2d, core):
    return np.ascontiguousarray(np.concatenate([x2d[b * 128:(b + 1) * 128] for b in GBLK[core]], axis=0))


def t5_bucket(rel):
    rel = np.asarray(rel, np.int64)
    n, max_exact = 16, 8
    ret = np.where(rel > 0, n, 0)
    a = np.abs(rel)
    af = np.maximum(a, 1).astype(np.float32)
    large = max_exact + (np.log(af / np.float32(max_exact)).astype(np.float32) / np.float32(math.log(128 / 8))
                         * np.float32(n - max_exact)).astype(np.int32)
    large = np.minimum(large, n - 1)
    return ret + np.where(a < max_exact, a, large)


def make_bidx():
    k = np.arange(128)[:, None]
    q = np.arange(128)[None, :]
    out = np.zeros((128, 2, 128), np.float32)
    diag = t5_bucket(k - q).astype(np.float32)
    diag[(k // 64) > (q // 64)] = 32.0
    out[:, 0, :] = diag
    out[:, 1, :] = t5_bucket(k - q - 128).astype(np.float32)
    return out


def make_farmask(core):
    fm = np.zeros((128, T, 64), np.float32)
    for t in range(T):
        b = GBLK[core][t]
        for j in range(64):
            if j > b - 2:
                fm[:, t, j] = NEG
    return fm


def make_near(core, kts, vs):
    nk = np.zeros((T, 6, 128, 2, 2, 128), kts[0].dtype)
    nv = np.zeros((T, 6, 128, 2, 257), vs[0].dtype)
    for t in range(T):
        b = GBLK[core][t]
        nk[t, :, :, 0] = kts[core][t]
        nv[t, :, :, 0] = np.transpose(vs[core][t], (1, 0, 2))
        if b >= 1:
            r, tt = OWNER[b - 1]
            nk[t, :, :, 1] = kts[r][tt]
            nv[t, :, :, 1] = np.transpose(vs[r][tt], (1, 0, 2))
    return nk, nv


def make_global_kv(kts, vs):
    gk = np.zeros((6, 128, 64, 2, 128), kts[0].dtype)
    gv = np.zeros((6, 128, 64, 257), vs[0].dtype)
    for r in range(NCORES):
        for t in range(T):
            j = GBLK[r][t]
            gk[:, :, j] = kts[r][t]
            gv[:, :, j] = np.transpose(vs[r][t], (1, 0, 2))
    return gk, gv


def _router_arrays(inputs, ls):
    wr = np.concatenate([inputs["moe_wg1"][ls], np.concatenate([inputs["moe_wg2"][ls, g] for g in range(4)], axis=-1)], axis=-1)
    br = np.concatenate([inputs["moe_bg1"][ls], inputs["moe_bg2"][ls].reshape(len(ls), 32)], axis=-1)
    return np.ascontiguousarray(wr, np.float32), np.ascontiguousarray(br, np.float32)


_PROGS = {}


def _prog(mode):
    if mode not in _PROGS:
        _PROGS[mode] = build_program(mode)[0]
    return _PROGS[mode]


def common_maps(inputs, ls):
    wr, br = _router_arrays(inputs, ls)
    m = {"mem": np.ascontiguousarray(inputs["mem"][0]), "ln_g": np.ascontiguousarray(inputs["ln_g"][ls]),
            "ln_b": np.ascontiguousarray(inputs["ln_b"][ls]), "mem_w_kv": np.ascontiguousarray(inputs["mem_w_kv"][ls]),
            "moe_wr": wr, "moe_br": br}
    for i in range(len(ls) // 2):
        sub = ls[2 * i:2 * i + 2]
        for nm in ("moe_w1", "moe_w3", "moe_w2"):
            m["%s_%d" % (nm, i)] = np.ascontiguousarray(inputs[nm][sub])
    return m


def maps_A(inputs, cores):
    cm = common_maps(inputs, [0, 1])
    x2d = np.asarray(inputs["x"][0], np.float32)
    out = []
    for cidx in cores:
        m = dict(cm)
        m.update({"x": shard_tokens(x2d, cidx), "consts": make_consts(cidx)})
        for k in ("a_w_in", "a_sgu_ln_g", "a_sgu_ln_b", "a_ws", "a_bs", "a_w_out", "shared_w_kv"):
            m[k] = np.ascontiguousarray(inputs[k], np.float32)
        out.append(m)
    return out


def maps_B(inputs, cores, xs, kts, vs):
    cm = common_maps(inputs, [2, 3])
    gk, gv = make_global_kv(kts, vs)
    bidx = make_bidx()
    out = []
    for n_, cidx in enumerate(cores):
        m = dict(cm)
        nk, nv = make_near(cidx, kts, vs)
        m.update({"x": xs[n_], "consts": make_consts(cidx), "g_k": gk, "g_v": gv, "near_k": nk, "near_v": nv,
                  "farmask": make_farmask(cidx), "bidx": bidx})
        for k in ("b_w_in", "b_lambda", "b_subln_g", "b_w_out", "rel_bias"):
            m[k] = np.ascontiguousarray(inputs[k], np.float32)
        out.append(m)
    return out


def make_prev(core):
    p = np.arange(128, dtype=np.int64)
    idx = np.zeros((128, T, 6), np.uint32)
    nm = np.zeros((128, T), np.float32)
    for t in range(T):
        b = GBLK[core][t]
        if b >= 1:
            r, tt = OWNER[b - 1]
        else:
            r, tt = core, t
            nm[:, t] = NEG
        for h_ in range(6):
            idx[:, t, h_] = ((r * T + tt) * 6 + h_) * 128 + p
    return idx, nm


def maps_F(inputs, cores):
    cm = common_maps(inputs, [0, 1, 2, 3])
    x2d = np.asarray(inputs["x"][0], np.float32)
    bidx = make_bidx()
    out = []
    for cidx in cores:
        m = dict(cm)
        pi, nm = make_prev(cidx)
        m.update({"x": shard_tokens(x2d, cidx), "consts": make_consts(cidx), "farmask": make_farmask(cidx), "bidx": bidx,
                  "previdx": pi, "nearmask": nm})
        for k in ("a_w_in", "a_sgu_ln_g", "a_sgu_ln_b", "a_ws", "a_bs", "a_w_out", "shared_w_kv",
                  "b_w_in", "b_lambda", "b_subln_g", "b_w_out", "rel_bias"):
            m[k] = np.ascontiguousarray(inputs[k], np.float32)
        out.append(m)
    return out


def kernel(**inputs):
    inputs = {k: np.asarray(v) for k, v in inputs.items()}
    cores = list(range(NCORES))
    resA = run_bass_kernel_spmd(_prog("A"), maps_A(inputs, cores), core_ids=cores).results
    xs = [np.asarray(r["y"], np.float32) for r in resA]
    kts = [np.asarray(r["kt"]) for r in resA]
    vs = [np.asarray(r["v"]) for r in resA]
    resB = run_bass_kernel_spmd(_prog("B"), maps_B(inputs, cores, xs, kts, vs), core_ids=cores).results
    out = np.zeros((8192, D), np.float32)
    for cidx in cores:
        y = np.asarray(resB[cidx]["y"], np.float32)
        for t in range(T):
            b = GBLK[cidx][t]
            out[b * 128:(b + 1) * 128] = y[t * 128:(t + 1) * 128]
    return out.reshape(1, 8192, D)
```

```python
import math
import numpy as np
import ml_dtypes
from contextlib import ExitStack
from concourse.bass_utils import run_bass_kernel_spmd

import concourse.bass as bass
import concourse.mybir as mybir

F32 = mybir.dt.float32
BF16 = mybir.dt.bfloat16
U32 = mybir.dt.uint32
I32 = mybir.dt.int32
AF = mybir.ActivationFunctionType
ALU = mybir.AluOpType
AX = mybir.AxisListType

ENGS = ("pe", "act", "dve", "pool", "sp")


class Buf:
    __slots__ = ("name", "writes", "reads", "sem", "semval", "base")

    def __init__(self, name):
        self.name = name
        self.writes = []
        self.reads = []
        self.base = None
        self.sem = None
        self.semval = 0


class _Rec:
    def __init__(self):
        self.call = None

    def __getattr__(self, name):
        def f(*a, **k):
            self.call = (name, a, k)
            return self
        return f


class Ins:
    __slots__ = ("eng", "fn", "waits", "flag", "seq", "semval", "dma", "noncontig")

    def __init__(self, eng, fn):
        self.eng = eng
        self.noncontig = False
        if fn is not None:
            rec = _Rec()
            fn(rec)
            assert rec.call is not None
            self.fn = rec.call
        else:
            self.fn = None
        self.waits = []
        self.flag = False
        self.seq = 0
        self.semval = None
        self.dma = None


class Prog:
    def __init__(self, nc, stack, same_engine_sync=True):
        self.nc = nc
        self.stack = stack
        self.lists = {e: [] for e in ENGS}
        self.esem = {e: stack.enter_context(nc.semaphore("es_" + e)) for e in ENGS}
        self.waited_eng = {e: {} for e in ENGS}
        self.waited_dma = {e: {} for e in ENGS}
        self.same = same_engine_sync
        self.nbuf = 0
        self.dma_sems = []

    def buf(self, name=None):
        self.nbuf += 1
        return Buf(name or "b%d" % self.nbuf)

    def share(self, bufs):
        sem = self.stack.enter_context(self.nc.semaphore("dg%d_%s" % (len(self.dma_sems), bufs[0].name)))
        self.dma_sems.append(sem)
        g = [sem, 0]
        for b in bufs:
            b.sem = g

    def fence(self, bufs):
        for e in ENGS:
            self.final_wait(e, bufs)

    def bufs(self, n, name="b"):
        return [self.buf("%s%d" % (name, i)) for i in range(n)]

    def _add_wait(self, ins, ev):
        e = ins.eng
        if ev[0] == "c":
            src = ev[1]
            if src.eng == e and (e == "pe" or not self.same):
                return
            w = self.waited_eng[e]
            if w.get(src.eng, -1) >= src.seq:
                return
            w[src.eng] = src.seq
            src.flag = True
            ins.waits.append(ev)
        else:
            _, sem, val = ev
            w = self.waited_dma[e]
            k = id(sem)
            if w.get(k, -1) >= val:
                return
            w[k] = val
            ins.waits.append(ev)

    def emit(self, eng, fn, reads=(), writes=(), cwrites=(), dma_dst=None, noncontig=False):
        ins = Ins(eng, fn)
        ins.noncontig = noncontig
        lst = self.lists[eng]
        ins.seq = len(lst)
        deps = []
        for b in reads:
            deps.extend(b.writes)
        for b in writes:
            deps.extend(b.writes)
            deps.extend(b.reads)
        for b in cwrites:
            deps.extend(b.reads)
            if b.base is not None:
                deps.append(b.base)
        if len(deps) > 1:
            deps = self._compact(deps)
        for ev in deps:
            self._add_wait(ins, ev)
        if dma_dst is not None:
            if dma_dst.sem is None:
                sem = self.stack.enter_context(self.nc.semaphore("ds%d_%s" % (len(self.dma_sems), dma_dst.name)))
                self.dma_sems.append(sem)
                dma_dst.sem = [sem, 0]
            g = dma_dst.sem
            g[1] += 16
            ins.dma = (g[0], g[1])
            ev = ("d", g[0], g[1])
        else:
            ev = ("c", ins)
        for b in writes:
            b.writes = [ev]
            b.reads = []
            b.base = ev
        for b in cwrites:
            b.writes.append(ev)
            if len(b.writes) > 24:
                b.writes = self._compact(b.writes)
        for b in reads:
            b.reads.append(ev)
            if len(b.reads) > 24:
                b.reads = self._compact(b.reads)
        lst.append(ins)
        return ins

    @staticmethod
    def _compact(evs):
        best = {}
        for ev in evs:
            if ev[0] == "c":
                k = ("c", ev[1].eng)
                if k not in best or best[k][1].seq < ev[1].seq:
                    best[k] = ev
            else:
                k = ("d", id(ev[1]))
                if k not in best or best[k][2] < ev[2]:
                    best[k] = ev
        return list(best.values())

    def barrier(self, bufs):
        pass

    def final_wait(self, eng, bufs):
        ins = Ins(eng, None)
        ins.seq = len(self.lists[eng])
        for b in bufs:
            for ev in b.writes:
                self._add_wait(ins, ev)
        self.lists[eng].append(ins)

    def finalize(self):
        nc = self.nc
        for e in ENGS:
            c = 0
            for ins in self.lists[e]:
                if ins.flag:
                    c += 1
                    ins.semval = c
        engobj = {"pe": "tensor", "act": "scalar", "dve": "vector", "pool": "gpsimd", "sp": "sync"}
        stats = {}
        with nc.Block() as block:
            for e in ENGS:
                lst = self.lists[e]
                esem = self.esem

                def body(eng, lst=lst, e=e):
                    nw = 0
                    for ins in lst:
                        for ev in ins.waits:
                            if ev[0] == "c":
                                eng.wait_ge(esem[ev[1].eng], ev[1].semval)
                            else:
                                eng.wait_ge(ev[1], ev[2])
                            nw += 1
                        if ins.fn is None:
                            continue
                        name, a, k = ins.fn
                        if ins.noncontig:
                            with nc.allow_non_contiguous_dma(reason="tiny strided load"):
                                r = getattr(eng, name)(*a, **k)
                        else:
                            r = getattr(eng, name)(*a, **k)
                        if ins.dma is not None:
                            r.then_inc(ins.dma[0], 16)
                            assert not ins.flag
                        elif ins.flag:
                            r.then_inc(esem[e], 1)
                    stats[e] = (len(lst), nw)

                getattr(block, engobj[e])(body)
        return stats


NCORES = 8
T = 8
NT = 1024
D = 2048
CAP = 128
NSLOT = 32 * CAP
ALPHA = float(8 ** 0.25)
EPS = 1e-5
SCALE = float(128 ** -0.5)
NEG = -30000.0


def zigzag_blocks(c):
    out = []
    for p in range(4):
        out.append(16 * p + c)
        out.append(16 * p + 15 - c)
    return out


GBLK = [zigzag_blocks(c) for c in range(NCORES)]
LT = [max(GBLK[c][t] for c in range(NCORES)) + 1 for t in range(T)]
OWNER = {}
for _c in range(NCORES):
    for _t in range(T):
        OWNER[GBLK[_c][_t]] = (_c, _t)


class Ctx:
    def __init__(self, nc, st):
        self.nc, self.st = nc, st
        self.P = Prog(nc, st)
        self.ps = [st.enter_context(nc.psum_tensor("ps%d" % i, [128, 512], F32)) for i in range(8)]
        self.bps = self.P.bufs(8, "ps")
        self.psi = 0
        self.evi = 0
        self.tmpi = 0

    def next_ps(self):
        i = self.psi
        self.psi = (i + 1) % 8
        return self.ps[i], self.bps[i]

    def sb(self, name, shape, dt):
        return self.st.enter_context(self.nc.sbuf_tensor(name, shape, dt))

    def dram(self, name, shape, dt, kind="Internal"):
        return self.nc.dram_tensor(name, shape, dt, kind=kind).ap()

    def E(self, eng, fn, r=(), w=(), cw=(), dma=None, nonc=False):
        return self.P.emit(eng, fn, reads=r, writes=w, cwrites=cw, dma_dst=dma, noncontig=nonc)

    def evac(self, out, in_, r, w=(), cw=()):
        self.evi += 1
        if self.evi % 2 == 0:
            self.E("dve", lambda e: e.tensor_copy(out=out, in_=in_), r, w, cw)
        else:
            self.E("act", lambda e: e.activation(out=out, in_=in_, func=AF.Copy), r, w, cw)


def build_program(mode, nlayers=2, debug=False):
    nc = bass.Bass("TRN2", target_bir_lowering=False)
    st = ExitStack()
    c = Ctx(nc, st)
    P = c.P
    E = c.E
    IN = lambda name, shape, dt=F32: nc.dram_tensor(name, shape, dt, kind="ExternalInput").ap()
    OUT = lambda name, shape, dt=F32: nc.dram_tensor(name, shape, dt, kind="ExternalOutput").ap()

    x_in = IN("x", [NT, D])
    mem = IN("mem", [256, D])
    consts = IN("consts", [128, 448])
    fused = (mode == "F")
    NL = 4 if fused else 2
    ln_g = IN("ln_g", [NL, 2, D])
    ln_b = IN("ln_b", [NL, 2, D])
    mem_w_kv = IN("mem_w_kv", [NL, D, 1024])
    moe_wr = IN("moe_wr", [NL, D, 36])
    moe_br = IN("moe_br", [NL, 36])
    _w1 = [IN("moe_w1_%d" % i, [2, 32, D, 512]) for i in range(NL // 2)]
    _w3 = [IN("moe_w3_%d" % i, [2, 32, D, 512]) for i in range(NL // 2)]
    _w2 = [IN("moe_w2_%d" % i, [2, 32, 512, D]) for i in range(NL // 2)]

    class _Split:
        def __init__(self, parts):
            self.parts = parts

        def __getitem__(self, key):
            l_, ex_ = key
            return self.parts[l_ // 2][l_ % 2, ex_]

    moe_w1, moe_w3, moe_w2 = _Split(_w1), _Split(_w3), _Split(_w2)
    if mode in ("A", "F"):
        a_w_in = IN("a_w_in", [2, D, 3584])
        a_sgu_ln_g = IN("a_sgu_ln_g", [2, 1536])
        a_sgu_ln_b = IN("a_sgu_ln_b", [2, 1536])
        a_ws = IN("a_ws", [2, 4, 128, 128])
        a_bs = IN("a_bs", [2, 4, 128])
        a_w_out = IN("a_w_out", [2, D, D])
        shared_w_kv = IN("shared_w_kv", [D, 3072])
        y_out = OUT("y", [NT, D])
        if not fused:
            kt_out = OUT("kt", [T, 6, 128, 2, 128], BF16)
            v_out = OUT("v", [T, 128, 6, 257], BF16)
        layers = [0, 1][:nlayers]
        if debug:
            dbg_slots = OUT("dbg_slots", [128, T, 2], U32)
            dbg_gates = OUT("dbg_gates", [128, T, 2], F32)
            dbg_xbuf = OUT("dbg_xbuf", [NSLOT + 128, D], BF16)
            dbg_ybuf = OUT("dbg_ybuf", [NSLOT + 128, D], BF16)
            dbg_xm = OUT("dbg_xm", [NT, D], F32)
            dbg_catT = OUT("dbg_catT", [128, 16, NT], BF16)
            dbg_vv = OUT("dbg_vv", [128, T * 1536], BF16)
    if mode in ("B", "F"):
        b_w_in = IN("b_w_in", [2, D, D])
        b_lambda = IN("b_lambda", [2, 4, 128])
        b_subln_g = IN("b_subln_g", [2, 256])
        b_w_out = IN("b_w_out", [2, D, D])
        rel_bias = IN("rel_bias", [32, 6])
        if fused:
            ktl2 = c.dram("ktl2", [T * 6 * 128, 256], BF16)
            vl2 = c.dram("vl2", [T * 6 * 128, 257], BF16)
            gk2 = c.dram("gk2", [NCORES * T * 6 * 128, 256], BF16)
            gv2 = c.dram("gv2", [NCORES * T * 6 * 128, 257], BF16)
            kt_out = ktl2.rearrange("(t h d) (m k) -> t h d m k", t=T, h=6, m=2)
            vl_v = vl2.rearrange("(t h k) e -> t h k e", t=T, h=6)
            g_k = gk2.rearrange("(r t h d) (m k) -> r t h d m k", r=NCORES, t=T, h=6, m=2)
            g_v = gv2.rearrange("(r t h k) e -> r t h k e", r=NCORES, t=T, h=6)
            previdx = IN("previdx", [128, T, 6], U32)
            nearmask = IN("nearmask", [128, T])
        else:
            g_k = IN("g_k", [6, 128, 64, 2, 128], BF16)
            g_v = IN("g_v", [6, 128, 64, 257], BF16)
            near_k = IN("near_k", [T, 6, 128, 2, 2, 128], BF16)
            near_v = IN("near_v", [T, 6, 128, 2, 257], BF16)
            y_out = OUT("y", [NT, D])
        farmask = IN("farmask", [128, T, 64])
        bidx = IN("bidx", [128, 2, 128])
        layers = [0, 1, 2, 3] if fused else [2, 3][:nlayers]
        if debug:
            dbg_catT = OUT("dbg_catT", [128, 16, NT], BF16)
            dbg_tbl = OUT("dbg_tbl", [128, 1536], BF16)
            dbg_qh = OUT("dbg_qh", [128, 2048], BF16)
            dbg_lam = OUT("dbg_lam", [128, 8], F32)

    L0 = layers[0]
    xm_d = c.dram("xm_d", [NT, D], F32)
    x_d = c.dram("x_d", [NT, D], F32)
    xbuf = c.dram("xbuf", [NSLOT + 128, D], BF16)
    ybuf = c.dram("ybuf", [NSLOT + 128, D], BF16)
    b_xm_d = P.bufs(T, "xmd")
    b_x_d = P.bufs(T, "xd")
    b_xbuf = P.buf("xbuf")
    b_ybuf = P.bufs(32, "ybuf")
    b_y = P.bufs(T, "yout")
    for grp in (b_xm_d, b_x_d, b_y):
        P.share(grp[0::2])
        P.share(grp[1::2])
    P.share(b_ybuf[0::2])
    P.share(b_ybuf[1::2])

    pool = c.sb("wpool", [128, 6 * 8192], BF16)
    bw = P.bufs(6, "slab")

    def slab(i):
        return pool[:, i * 8192:(i + 1) * 8192]

    def slab_in(i):
        return slab(i).rearrange("p (k n) -> p k n", k=16)

    def slab_w2(i):
        return slab(i).rearrange("p (k n) -> p k n", k=4)

    xT = pool[:, 2 * 8192:4 * 8192].rearrange("p (k n) -> p k n", k=16)
    catT = pool[:, 4 * 8192:6 * 8192].rearrange("p (k n) -> p k n", k=16)
    b_xT = P.bufs(T, "xT")
    b_catT = P.bufs(T, "catT")
    XG = [bw[2], bw[3]]
    CG = [bw[4], bw[5]]

    cst = c.sb("cst", [128, 448], F32)
    b_cst = P.buf("cst")
    identb = c.sb("identb", [128, 128], BF16)
    ustrb = c.sb("ustrb", [128, 128], BF16)
    onesb = c.sb("onesb", [128, 128], BF16)
    b_cb = P.buf("cb")
    identf = cst[:, 0:128]
    sgumask = cst[:, 256:384]
    ebase = cst[:, 384:416]
    trashc = cst[:, 416:417]

    gB = c.sb("gB", [128, D], F32)
    bB = c.sb("bB", [128, D], F32)
    b_gB = P.buf("gB")
    xw = [c.sb("xw%d" % i, [128, D], F32) for i in range(2)]
    b_xw = P.bufs(2, "xw")
    kmT = c.sb("kmT", [128, 4, 256], BF16)
    vm = c.sb("vm", [128, 2, 4, 129], BF16)
    b_kmT = P.buf("kmT")
    b_vm = P.buf("vm")
    qmT = c.sb("qmT", [128, 4, NT], BF16)
    b_qmT = P.buf("qmT")
    stt = c.sb("stt", [128, T, 4, 6], F32)
    mvt = c.sb("mvt", [128, T, 2], F32)
    rst = c.sb("rst", [128, T, 4], F32)
    b_st = P.bufs(T, "st")
    wr = c.sb("wr", [128, 16, 36], F32)
    brB = c.sb("brB", [128, 36], F32)
    b_wr = P.buf("wr")
    xTf_t = c.sb("xTf", [128, 16, 128], F32)
    xTf = [xTf_t, xTf_t]
    _bx = P.buf("xTf")
    b_xTf = [_bx, _bx]
    rt = c.sb("rt", [128, T, 160], F32)
    b_rt = P.bufs(T, "rt")
    gates = c.sb("gates", [128, T, 2], F32)
    slots = c.sb("slots", [128, T, 2], U32)
    Ab = c.sb("Ab", [128, T, 32], BF16)
    b_Ab = P.bufs(T, "Ab")
    b_gs = P.bufs(T, "gs")
    xbf_t = c.sb("xbf", [128, D], BF16)
    xbf = [xbf_t, xbf_t]
    _bx2 = P.buf("xbf")
    b_xbf = [_bx2, _bx2]
    c.vreg = c.sb("vreg", [128, 16896], BF16)
    vr = c.vreg
    xe = [vr[:, i * 2048:(i + 1) * 2048] for i in range(2)]
    b_xe = P.bufs(2, "xe")
    xeT = [vr[:, 4096 + i * 2048:4096 + (i + 1) * 2048].rearrange("p (k n) -> p k n", k=16) for i in range(2)]
    b_xeT = P.bufs(2, "xeT")
    hs = [vr[:, 8192 + i * 1024:8192 + (i + 1) * 1024].bitcast(F32) for i in range(2)]
    hb = [vr[:, 10240 + i * 512:10240 + (i + 1) * 512] for i in range(2)]
    hT = [vr[:, 11264 + i * 512:11264 + (i + 1) * 512].rearrange("p (k n) -> p k n", k=4) for i in range(2)]
    b_hs = P.bufs(2, "hs")
    b_hb = P.bufs(2, "hb")
    b_hT = P.bufs(2, "hT")
    ye = [vr[:, 12288 + i * 2048:12288 + (i + 1) * 2048] for i in range(2)]
    b_ye = P.bufs(2, "ye")
    y01_t = xTf_t[:].rearrange("p a b -> p (a b)").bitcast(BF16).rearrange("p (a b) -> p a b", a=2)
    y01 = [y01_t, y01_t]
    _by = P.buf("y01")
    b_y01 = [_by, _by]
    catc = [c.sb("catc%d" % i, [128, 512], BF16) for i in range(3)]
    b_catc = P.bufs(3, "catc")
    pT = [c.sb("pT%d" % i, [128, 512], BF16) for i in range(3)]
    b_pT = P.bufs(3, "pT")
    rc = c.sb("rc", [128, 16], F32)
    b_rc = P.bufs(16, "rc")
    cnt = {"catc": 0, "pT": 0, "rc": 0, "ug": 0}

    def rot(name, n):
        i = cnt[name]
        cnt[name] = (i + 1) % n
        return i

    E("sp", lambda e: e.dma_start(out=cst[:], in_=consts), w=[b_cst], dma=b_cst)
    E("dve", lambda e: e.tensor_copy(out=identb[:], in_=cst[:, 0:128]), r=[b_cst], w=[b_cb])
    E("dve", lambda e: e.tensor_copy(out=ustrb[:], in_=cst[:, 128:256]), r=[b_cst], cw=[b_cb])
    E("dve", lambda e: e.memset(onesb[:], 1.0), cw=[b_cb])
    E("dve", lambda e: e.memset(vm[:, :, :, 128:129], 1.0), w=[b_vm])

    E("dve", lambda e: e.memset(ye[0], 0.0), w=[b_ye[0]])
    E("sp", lambda e: e.dma_start(out=ybuf[NSLOT:NSLOT + 128, :], in_=ye[0]), r=[b_ye[0]], w=[b_ybuf[0]], dma=b_ybuf[0])

    def transpose_tile_f32(src, b_src, dst_fn, b_dst_w, b_dst_cw=(), dst_dt_copy=True):
        for g in range(4):
            ps, bp = c.next_ps()
            for j in range(4):
                k = 4 * g + j
                E("pe", lambda e, ps=ps, j=j, k=k: e.transpose(out=ps[:, j * 128:(j + 1) * 128],
                                                          in_=src[:, k * 128:(k + 1) * 128], identity=identf),
                  r=[b_src, b_cst], w=[bp] if j == 0 else (), cw=() if j == 0 else [bp])
            c.evac(dst_fn(g), ps[:].rearrange("p (a b) -> p a b", a=4), r=[bp],
                   w=b_dst_w if g == 0 else (), cw=list(b_dst_cw) + (list(b_dst_w) if g else []))

    def make_xT(src, b_src, t):
        transpose_tile_f32(src, b_src, lambda g, t=t: xT[:, 4 * g:4 * g + 4, t * 128:(t + 1) * 128],
                           [b_xT[t]], XG)

    if True:
        for t in range(T):
            i = t % 2
            E("sp", lambda e, i=i, t=t: e.dma_start(out=xw[i][:], in_=x_in[t * 128:(t + 1) * 128, :]),
              w=[b_xw[i]], dma=b_xw[i])
            make_xT(xw[i], b_xw[i], t)

    def load_slab_in(i, W, c0, ncols=512):
        dst = slab(i)[:, 0:16 * ncols].rearrange("p (k n) -> p k n", k=16)
        E("pool", lambda e: e.dma_start(out=dst, in_=W[:, c0:c0 + ncols].rearrange("(k p) n -> p k n", p=128)),
          w=[bw[i]], dma=bw[i])
        return dst

    def bcast_load(dst, vec, b_dst, n):
        E("sp", lambda e: e.dma_start(out=dst, in_=vec.partition_broadcast(128)), w=[b_dst], dma=b_dst)

    def ln_tile(xt, b_xt, t, width, gap, bap, b_gb, out_ap=None, b_out=None, sti=0):
        nch = width // 512
        for j in range(nch):
            E("dve", lambda e, j=j: e.bn_stats(out=stt[:, t, j, :], in_=xt[:, j * 512:(j + 1) * 512]),
              r=[b_xt], w=[b_st[t]] if j == 0 else (), cw=() if j == 0 else [b_st[t]])
        E("dve", lambda e: e.bn_aggr(out=mvt[:, t, :], in_=stt[:, t, 0:nch, :]), r=[b_st[t]], cw=[b_st[t]])
        E("dve", lambda e: e.tensor_scalar(out=rst[:, t, 0:1], in0=mvt[:, t, 1:2], scalar1=EPS, scalar2=None,
                                           op0=ALU.add), r=[b_st[t]], cw=[b_st[t]])
        E("act", lambda e: e.activation(out=rst[:, t, 1:2], in_=rst[:, t, 0:1], func=AF.Ln), r=[b_st[t]], cw=[b_st[t]])
        E("act", lambda e: e.activation(out=rst[:, t, 2:3], in_=rst[:, t, 1:2], func=AF.Exp, scale=-0.5), r=[b_st[t]], cw=[b_st[t]])
        E("dve", lambda e: e.tensor_scalar(out=rst[:, t, 3:4], in0=mvt[:, t, 0:1], scalar1=rst[:, t, 2:3], scalar2=-1.0,
                                           op0=ALU.mult, op1=ALU.mult), r=[b_st[t]], cw=[b_st[t]])
        o = xt if out_ap is None else out_ap
        E("act", lambda e: e.activation(out=xt, in_=xt, func=AF.Identity, scale=rst[:, t, 2:3], bias=rst[:, t, 3:4]),
          r=[b_st[t]], w=[b_xt])
        E("dve", lambda e: e.tensor_tensor(out=xt, in0=xt, in1=gap, op=ALU.mult), r=[b_gb], w=[b_xt])
        if out_ap is None:
            E("dve", lambda e: e.tensor_tensor(out=xt, in0=xt, in1=bap, op=ALU.add), r=[b_gb], w=[b_xt])
        else:
            E("dve", lambda e: e.tensor_tensor(out=out_ap, in0=xt, in1=bap, op=ALU.add), r=[b_gb, b_xt], w=[b_out])

    def mem_kv(l):
        sK = load_slab_in(0, mem_w_kv[l - L0], 0)
        sV = load_slab_in(1, mem_w_kv[l - L0], 512)
        memT = xw[1][:].bitcast(BF16).rearrange("p (k n) -> p k n", k=16)
        b_memT = b_xw[1]
        for kc in range(2):
            E("sp", lambda e, kc=kc: e.dma_start(out=xw[0][:], in_=mem[kc * 128:(kc + 1) * 128, :]), w=[b_xw[0]], dma=b_xw[0])
            transpose_tile_f32(xw[0], b_xw[0], lambda g, kc=kc: memT[:, 4 * g:4 * g + 4, kc * 128:(kc + 1) * 128],
                               [b_memT] if kc == 0 else [], [] if kc == 0 else [b_memT])
        for h in range(4):
            ps, bp = c.next_ps()
            for k in range(16):
                E("pe", lambda e, ps=ps, k=k, h=h: e.matmul(ps[:, 0:256], lhsT=sK[:, k, h * 128:(h + 1) * 128],
                                                          rhs=memT[:, k, :], start=(k == 0), stop=(k == 15)),
                  r=[bw[0], b_memT], w=[bp] if k == 0 else (), cw=() if k == 0 else [bp])
            c.evac(kmT[:, h, :], ps[:, 0:256], r=[bp], w=[b_kmT] if h == 0 else (), cw=() if h == 0 else [b_kmT])
        for kc in range(2):
            ps, bp = c.next_ps()
            for k in range(16):
                E("pe", lambda e, ps=ps, k=k, kc=kc: e.matmul(ps[:], lhsT=memT[:, k, kc * 128:(kc + 1) * 128],
                                                            rhs=sV[:, k, :], start=(k == 0), stop=(k == 15)),
                  r=[bw[1], b_memT], w=[bp] if k == 0 else (), cw=() if k == 0 else [bp])
            c.evac(vm[:, kc, :, 0:128], ps[:].rearrange("p (h d) -> p h d", h=4), r=[bp], cw=[b_vm])

    def q_featmajor(sl, b_sl, ncol_chunks, dstfn, b_dst):
        first = True
        for fc in range(ncol_chunks):
            for half in range(2):
                ps, bp = c.next_ps()
                for k in range(16):
                    E("pe", lambda e, ps=ps, k=k, fc=fc, half=half: e.matmul(
                        ps[:], lhsT=sl[:, k, fc * 128:(fc + 1) * 128], rhs=xT[:, k, half * 512:(half + 1) * 512],
                        start=(k == 0), stop=(k == 15)),
                      r=[b_sl] + b_xT[4 * half:4 * half + 4] + XG, w=[bp] if k == 0 else (), cw=() if k == 0 else [bp])
                c.evac(dstfn(fc, half), ps[:], r=[bp], w=[b_dst] if first else (), cw=() if first else [b_dst])
                first = False

    def mem_attn_tile(t):
        ci = rot("catc", 3)
        cc, bcc = catc[ci], b_catc[ci]
        for h in range(4):
            ps, bp = c.next_ps()
            for kc in range(2):
                E("pe", lambda e, ps=ps, kc=kc, h=h: e.matmul(ps[:, kc * 128:(kc + 1) * 128],
                                                            lhsT=kmT[:, h, kc * 128:(kc + 1) * 128],
                                                            rhs=qmT[:, h, t * 128:(t + 1) * 128], start=True, stop=True),
                  r=[b_kmT, b_qmT], w=[bp] if kc == 0 else (), cw=() if kc == 0 else [bp])
            pi = rot("pT", 3)
            E("act", lambda e, ps=ps, pi=pi: e.activation(out=pT[pi][:, 0:256], in_=ps[:, 0:256], func=AF.Exp, scale=SCALE),
              r=[bp], w=[b_pT[pi]])
            ps2, bp2 = c.next_ps()
            for kc in range(2):
                E("pe", lambda e, ps2=ps2, kc=kc, h=h, pi=pi: e.matmul(ps2[:, 0:129], lhsT=pT[pi][:, kc * 128:(kc + 1) * 128],
                                                                     rhs=vm[:, kc, h, :], start=(kc == 0), stop=(kc == 1)),
                  r=[b_pT[pi], b_vm], w=[bp2] if kc == 0 else (), cw=() if kc == 0 else [bp2])
            ri = rot("rc", 16)
            E("dve", lambda e, ps2=ps2, ri=ri: e.reciprocal(out=rc[:, ri:ri + 1], in_=ps2[:, 128:129]), r=[bp2], w=[b_rc[ri]])
            E("dve", lambda e, ps2=ps2, ri=ri, h=h: e.tensor_scalar(out=cc[:, h * 128:(h + 1) * 128], in0=ps2[:, 0:128],
                                                                    scalar1=rc[:, ri:ri + 1], scalar2=None, op0=ALU.mult),
              r=[bp2, b_rc[ri]], w=[bcc] if h == 0 else (), cw=() if h == 0 else [bcc])
        cat_chunk_to_catT(cc, bcc, 12, t)

    def cat_chunk_to_catT(cc, bcc, k0, t, n=4):
        ps, bp = c.next_ps()
        psb = ps[:].bitcast(BF16)
        for j in range(n):
            E("pe", lambda e, j=j: e.transpose(out=psb[:, j * 128:(j + 1) * 128], in_=cc[:, j * 128:(j + 1) * 128],
                                               identity=identb[:]),
              r=[bcc, b_cb], w=[bp] if j == 0 else (), cw=() if j == 0 else [bp])
        c.evac(catT[:, k0:k0 + n, t * 128:(t + 1) * 128], psb[:, 0:n * 128].rearrange("p (a b) -> p a b", a=n),
               r=[bp], cw=[b_catT[t]] + CG)

    def mixer_A(l):
        i = l
        W = a_w_in[i]
        vv = c.vreg[:, 0:T * 1536].rearrange("p (t n) -> p t n", t=T)
        gS = gB[:, 0:1536]
        bS = bB[:, 0:1536]
        bcast_load(gS, a_sgu_ln_g[i], b_gB, 1536)
        E("sp", lambda e: e.dma_start(out=bS, in_=a_sgu_ln_b[i].partition_broadcast(128)), cw=[b_gB], dma=b_gB)
        wsf = xw[0][:, 0:512].rearrange("p (g j) -> p g j", g=4)
        E("sp", lambda e: e.dma_start(out=wsf, in_=a_ws[i].rearrange("g i j -> i g j")), w=[b_xw[0]], dma=b_xw[0])
        E("dve", lambda e: e.tensor_tensor(out=wsf, in0=wsf, in1=sgumask.unsqueeze(1).to_broadcast([128, 4, 128]),
                                           op=ALU.mult), r=[b_cst], w=[b_xw[0]])
        ps, bp = c.next_ps()
        for g in range(4):
            E("pe", lambda e, g=g: e.transpose(out=ps[:, g * 128:(g + 1) * 128], in_=xw[0][:, g * 128:(g + 1) * 128],
                                               identity=identf), r=[b_xw[0], b_cst],
              w=[bp] if g == 0 else (), cw=() if g == 0 else [bp])
        c.evac(c.wsT[:], ps[:].rearrange("p (g i) -> p g i", g=4), r=[bp], w=[c.b_wsT])
        E("sp", lambda e: e.dma_start(out=c.bsT[:], in_=a_bs[i].rearrange("g i -> i g")), w=[c.b_bsT], dma=c.b_bsT, nonc=True)

        for s in (3, 4, 5):
            sl = load_slab_in((s - 3) % 2, W, s * 512)
            for t in range(T):
                ps, bp = c.next_ps()
                for k in range(16):
                    E("pe", lambda e, ps=ps, k=k, t=t, sl=sl: e.matmul(ps[:], lhsT=xT[:, k, t * 128:(t + 1) * 128], rhs=sl[:, k, :],
                                                                      start=(k == 0), stop=(k == 15)),
                      r=[bw[(s - 3) % 2], b_xT[t]] + XG, w=[bp] if k == 0 else (), cw=() if k == 0 else [bp])
                E("act", lambda e, ps=ps, t=t, s=s: e.activation(out=vv[:, t, (s - 3) * 512:(s - 2) * 512], in_=ps[:], func=AF.Gelu),
                  r=[bp], w=[c.b_v[t]] if s == 3 else (), cw=() if s == 3 else [c.b_v[t]])
        sl = load_slab_in(1, W, 3072)
        q_featmajor(sl, bw[1], 4, lambda fc, half: qmT[:, fc, half * 512:(half + 1) * 512], b_qmT)
        for t in range(T):
            tmp = xw[t % 2][:, 0:1536]
            btmp = b_xw[t % 2]
            E("dve", lambda e, t=t, tmp=tmp: e.tensor_copy(out=tmp, in_=vv[:, t, :]), r=[c.b_v[t]], w=[btmp])
            ln_tile(tmp, btmp, t, 1536, gS, bS, b_gB, out_ap=vv[:, t, :], b_out=c.b_v[t])
        mem_kv(l)
        slabs_u = {0: 0, 1: 1, 2: 0}
        for s in (0, 1, 2):
            si = slabs_u[s]
            sl = load_slab_in(si, W, s * 512)
            for t in range(T):
                ps, bp = c.next_ps()
                for k in range(16):
                    E("pe", lambda e, ps=ps, k=k, t=t, sl=sl: e.matmul(ps[:], lhsT=xT[:, k, t * 128:(t + 1) * 128], rhs=sl[:, k, :],
                                                                      start=(k == 0), stop=(k == 15)),
                      r=[bw[si], b_xT[t]] + XG, w=[bp] if k == 0 else (), cw=() if k == 0 else [bp])
                ui = rot("pT", 3)
                ug = pT[ui]
                E("act", lambda e, ps=ps, ug=ug: e.activation(out=ug[:], in_=ps[:], func=AF.Gelu), r=[bp], w=[b_pT[ui]])
                psg, bpg = c.next_ps()
                ci = rot("catc", 3)
                cc, bcc = catc[ci], b_catc[ci]
                segs = []
                lo = s * 512
                while lo < (s + 1) * 512:
                    g = lo // 384
                    hi = min((g + 1) * 384, (s + 1) * 512)
                    segs.append((g, lo, hi))
                    lo = hi
                for n_, (g, lo, hi) in enumerate(segs):
                    E("pe", lambda e, psg=psg, g=g, lo=lo, hi=hi, t=t: e.matmul(psg[:, lo - s * 512:hi - s * 512], lhsT=c.wsT[:, g, :],
                                                                                rhs=vv[:, t, lo:hi], start=True, stop=True),
                      r=[c.b_wsT, c.b_v[t]], w=[bpg] if n_ == 0 else (), cw=() if n_ == 0 else [bpg])
                for n_, (g, lo, hi) in enumerate(segs):
                    E("dve", lambda e, psg=psg, g=g, lo=lo, hi=hi, cc=cc, ug=ug: e.scalar_tensor_tensor(
                        out=cc[:, lo - s * 512:hi - s * 512], in0=psg[:, lo - s * 512:hi - s * 512], scalar=c.bsT[:, g:g + 1],
                        in1=ug[:, lo - s * 512:hi - s * 512], op0=ALU.add, op1=ALU.mult),
                      r=[bpg, c.b_bsT, b_pT[ui]], w=[bcc] if n_ == 0 else (), cw=() if n_ == 0 else [bcc])
                cat_chunk_to_catT(cc, bcc, 4 * s, t)
                if s == 2:
                    mem_attn_tile(t)
        return a_w_out[i]

    def outproj_ln_route(l, w_out, x_src):
        sls = [load_slab_in(s, w_out, s * 512) for s in range(4)]
        bcast_load(gB[:], ln_g[l - L0, 0], b_gB, D)
        E("sp", lambda e: e.dma_start(out=bB[:], in_=ln_b[l - L0, 0].partition_broadcast(128)), cw=[b_gB], dma=b_gB)
        E("sp", lambda e: e.dma_start(out=wr[:], in_=moe_wr[l - L0].rearrange("(k p) n -> p k n", p=128)), w=[b_wr], dma=b_wr)
        E("sp", lambda e: e.dma_start(out=brB[:], in_=moe_br[l - L0].partition_broadcast(128)), cw=[b_wr], dma=b_wr)
        for t in range(T):
            i = t % 2
            xt, bx = xw[i], b_xw[i]
            E("sp", lambda e, xt=xt, t=t: e.dma_start(out=xt[:], in_=x_src[t * 128:(t + 1) * 128, :]),
              r=[b_x_d[t]] if x_src is not x_in else (), w=[bx], dma=bx)
            for s in range(4):
                ps, bp = c.next_ps()
                for k in range(16):
                    E("pe", lambda e, ps=ps, k=k, s=s, t=t: e.matmul(ps[:], lhsT=catT[:, k, t * 128:(t + 1) * 128], rhs=sls[s][:, k, :],
                                                                    start=(k == 0), stop=(k == 15)),
                      r=[bw[s], b_catT[t]] + CG, w=[bp] if k == 0 else (), cw=() if k == 0 else [bp])
                E("dve", lambda e, ps=ps, s=s, xt=xt: e.scalar_tensor_tensor(out=xt[:, s * 512:(s + 1) * 512], in0=xt[:, s * 512:(s + 1) * 512],
                                                                          scalar=ALPHA, in1=ps[:], op0=ALU.mult, op1=ALU.add),
                  r=[bp], w=[bx])
            ln_tile(xt[:], bx, t, D, gB[:], bB[:], b_gB)
            E("sp", lambda e, xt=xt, t=t: e.dma_start(out=xm_d[t * 128:(t + 1) * 128, :], in_=xt[:]), r=[bx], w=[b_xm_d[t]], dma=b_xm_d[t])
            route_tile(l, t, xt, bx)

    def route_tile(l, t, xt, bx):
        fi = t % 2
        xf, bxf = xTf[fi], b_xTf[fi]
        transpose_tile_f32(xt, bx, lambda g: xf[:, 4 * g:4 * g + 4, :], [bxf])
        ps, bp = c.next_ps()
        for k in range(16):
            E("pe", lambda e, k=k: e.matmul(ps[:, 0:36], lhsT=xf[:, k, :], rhs=wr[:, k, :], start=(k == 0), stop=(k == 15)),
              r=[bxf, b_wr], w=[bp] if k == 0 else (), cw=() if k == 0 else [bp])
        R = rt[:, t, :]
        br_ = b_rt[t]
        lg = R[:, 0:36]
        V = lambda e: e
        D_ = lambda fn, r=(), first=False: E("dve", fn, r=list(r) + [br_], w=[br_]) if not first else E("dve", fn, r=list(r), w=[br_])
        D_(lambda e: e.tensor_tensor(out=lg, in0=ps[:, 0:36], in1=brB[:], op=ALU.add), r=[bp, b_wr], first=True)
        m1 = R[:, 36:37]
        D_(lambda e: e.tensor_reduce(out=m1, in_=R[:, 0:4], axis=AX.X, op=ALU.max))
        oh1 = R[:, 40:44]
        D_(lambda e: e.tensor_scalar(out=oh1, in0=R[:, 0:4], scalar1=m1, scalar2=None, op0=ALU.is_equal))
        nm1 = R[:, 37:38]
        D_(lambda e: e.tensor_scalar(out=nm1, in0=m1, scalar1=-1.0, scalar2=None, op0=ALU.mult))
        e1 = R[:, 44:48]
        s1 = R[:, 38:39]
        E("act", lambda e: e.activation(out=e1, in_=R[:, 0:4], func=AF.Exp, bias=nm1, scale=1.0, accum_out=s1), r=[br_], w=[br_])
        psel = R[:, 39:40]
        D_(lambda e: e.reciprocal(out=psel, in_=s1))
        pen = R[:, 48:52]
        D_(lambda e: e.tensor_scalar(out=pen, in0=oh1, scalar1=1.0e9, scalar2=-1.0e9, op0=ALU.mult, op1=ALU.add))
        lg2 = R[:, 52:84]
        D_(lambda e: e.tensor_tensor(out=lg2.rearrange("p (g k) -> p g k", g=4), in0=R[:, 4:36].rearrange("p (g k) -> p g k", g=4),
                                     in1=pen.unsqueeze(2).to_broadcast([128, 4, 8]), op=ALU.add))
        top8 = R[:, 84:92]
        D_(lambda e: e.max(out=top8, in_=lg2))
        ohA = R[:, 92:124]
        D_(lambda e: e.tensor_scalar(out=ohA, in0=lg2, scalar1=top8[:, 0:1], scalar2=None, op0=ALU.is_equal))
        ohB = R[:, 124:156]
        D_(lambda e: e.tensor_scalar(out=ohB, in0=lg2, scalar1=top8[:, 1:2], scalar2=None, op0=ALU.is_equal))
        dd = R[:, 156:157]
        D_(lambda e: e.tensor_tensor(out=dd, in0=top8[:, 1:2], in1=top8[:, 0:1], op=ALU.subtract))
        ed = R[:, 157:158]
        E("act", lambda e: e.activation(out=ed, in_=dd, func=AF.Exp), r=[br_], w=[br_])
        D_(lambda e: e.tensor_scalar(out=ed, in0=ed, scalar1=1.0, scalar2=None, op0=ALU.add))
        sg = R[:, 158:159]
        D_(lambda e: e.reciprocal(out=sg, in_=ed))
        gA = gates[:, t, 0:1]
        gBt = gates[:, t, 1:2]
        E("dve", lambda e: e.tensor_tensor(out=gA, in0=sg, in1=psel, op=ALU.mult), r=[br_], w=[b_gs[t]])
        E("dve", lambda e: e.tensor_tensor(out=gBt, in0=psel, in1=gA, op=ALU.subtract), r=[br_], w=[b_gs[t]])
        E("dve", lambda e: e.tensor_tensor(out=Ab[:, t, :], in0=ohA, in1=ohB, op=ALU.add), r=[br_], w=[b_Ab[t]])
        psr, bpr = c.next_ps()
        E("pe", lambda e: e.matmul(psr[:, 0:32], lhsT=ustrb[:], rhs=Ab[:, t, :], start=True, stop=(t == 0)),
          r=[b_cb, b_Ab[t]], w=[bpr])
        for t2 in range(t):
            E("pe", lambda e, t2=t2: e.matmul(psr[:, 0:32], lhsT=onesb[:], rhs=Ab[:, t2, :], start=False, stop=(t2 == t - 1)),
              r=[b_cb, b_Ab[t2]], cw=[bpr])
        rk = R[:, 0:32]
        D_(lambda e: e.tensor_copy(out=rk, in_=psr[:, 0:32]), r=[bpr])
        for n_, (oh, gcol) in enumerate(((ohA, gA), (ohB, gBt))):
            sel = R[:, 36 + n_:37 + n_]
            prod = R[:, 52:84]
            D_(lambda e, oh=oh: e.tensor_tensor(out=prod, in0=oh, in1=rk, op=ALU.mult))
            D_(lambda e, sel=sel: e.tensor_reduce(out=sel, in_=prod, axis=AX.X, op=ALU.add))
            D_(lambda e, oh=oh: e.tensor_tensor(out=prod, in0=oh, in1=ebase, op=ALU.mult), r=[b_cst])
            base = R[:, 38 + n_:39 + n_]
            D_(lambda e, base=base: e.tensor_reduce(out=base, in_=prod, axis=AX.X, op=ALU.add))
            ok = R[:, 44 + n_:45 + n_]
            D_(lambda e, ok=ok, sel=sel: e.tensor_scalar(out=ok, in0=sel, scalar1=float(CAP) - 0.5, scalar2=None, op0=ALU.is_lt))
            sf = R[:, 46 + n_:47 + n_]
            D_(lambda e, sf=sf, base=base, sel=sel: e.tensor_tensor(out=sf, in0=base, in1=sel, op=ALU.add))
            D_(lambda e, sf=sf: e.tensor_tensor(out=sf, in0=sf, in1=trashc, op=ALU.subtract), r=[b_cst])
            D_(lambda e, sf=sf, ok=ok: e.tensor_tensor(out=sf, in0=sf, in1=ok, op=ALU.mult))
            D_(lambda e, sf=sf: e.tensor_tensor(out=sf, in0=sf, in1=trashc, op=ALU.add), r=[b_cst])
            E("dve", lambda e, sf=sf, n_=n_: e.tensor_copy(out=slots[:, t, n_:n_ + 1], in_=sf), r=[br_], w=[b_gs[t]] if False else (), cw=[b_gs[t]])
            E("dve", lambda e, gcol=gcol, ok=ok: e.tensor_tensor(out=gcol, in0=gcol, in1=ok, op=ALU.mult), r=[br_], cw=[b_gs[t]])
        xb, bxb = xbf[fi], b_xbf[fi]
        E("act", lambda e: e.activation(out=xb[:], in_=xt[:], func=AF.Copy), r=[bx], w=[bxb])
        for n_ in range(2):
            E("pool", lambda e, n_=n_: e.indirect_dma_start(out=xbuf, out_offset=bass.IndirectOffsetOnAxis(ap=slots[:, t, n_:n_ + 1], axis=0),
                                                            in_=xb[:], in_offset=None),
              r=[bxb, b_gs[t]], cw=[b_xbuf], dma=b_xbuf)

    def experts(l):
        for ex in range(32):
            s1, s3, s2 = [(3 * ex + j) % 6 for j in range(3)]
            w1 = load_slab_in(s1, moe_w1[l - L0, ex], 0)
            w3 = load_slab_in(s3, moe_w3[l - L0, ex], 0)
            w2v = slab_w2(s2)
            E("pool", lambda e, w2v=w2v, ex=ex: e.dma_start(out=w2v, in_=moe_w2[l - L0, ex].rearrange("(k p) n -> p k n", p=128)),
              w=[bw[s2]], dma=bw[s2])
            i = ex % 2
            E("sp", lambda e, i=i, ex=ex: e.dma_start(out=xe[i], in_=xbuf[ex * CAP:(ex + 1) * CAP, :]), r=[b_xbuf], w=[b_xe[i]], dma=b_xe[i])
            for g in range(2):
                ps, bp = c.next_ps()
                psb = ps[:].bitcast(BF16)
                for j in range(8):
                    k = 8 * g + j
                    E("pe", lambda e, psb=psb, j=j, k=k, i=i: e.transpose(out=psb[:, j * 128:(j + 1) * 128], in_=xe[i][:, k * 128:(k + 1) * 128],
                                                                        identity=identb[:]),
                      r=[b_xe[i], b_cb], w=[bp] if j == 0 else (), cw=() if j == 0 else [bp])
                c.evac(xeT[i][:, 8 * g:8 * g + 8, :], psb.rearrange("p (a b) -> p a b", a=8), r=[bp],
                       w=[b_xeT[i]] if g == 0 else (), cw=() if g == 0 else [b_xeT[i]])
            ps1, bp1 = c.next_ps()
            ps3, bp3 = c.next_ps()
            for (ps, bp, wv, sb_) in ((ps1, bp1, w1, s1), (ps3, bp3, w3, s3)):
                for k in range(16):
                    E("pe", lambda e, ps=ps, k=k, wv=wv, i=i: e.matmul(ps[:], lhsT=xeT[i][:, k, :], rhs=wv[:, k, :], start=(k == 0), stop=(k == 15)),
                      r=[b_xeT[i], bw[sb_]], w=[bp] if k == 0 else (), cw=() if k == 0 else [bp])
            E("act", lambda e, i=i: e.activation(out=hs[i], in_=ps1[:], func=AF.Silu), r=[bp1], w=[b_hs[i]])
            E("dve", lambda e, i=i: e.tensor_tensor(out=hb[i], in0=hs[i], in1=ps3[:], op=ALU.mult), r=[b_hs[i], bp3], w=[b_hb[i]])
            ps, bp = c.next_ps()
            psb = ps[:].bitcast(BF16)
            for j in range(4):
                E("pe", lambda e, psb=psb, j=j, i=i: e.transpose(out=psb[:, j * 128:(j + 1) * 128], in_=hb[i][:, j * 128:(j + 1) * 128], identity=identb[:]),
                  r=[b_hb[i], b_cb], w=[bp] if j == 0 else (), cw=() if j == 0 else [bp])
            c.evac(hT[i], psb[:, 0:512].rearrange("p (a b) -> p a b", a=4), r=[bp], w=[b_hT[i]])
            for n4 in range(4):
                ps, bp = c.next_ps()
                for k in range(4):
                    E("pe", lambda e, ps=ps, k=k, n4=n4, i=i: e.matmul(ps[:], lhsT=hT[i][:, k, :], rhs=w2v[:, k, n4 * 512:(n4 + 1) * 512],
                                                                      start=(k == 0), stop=(k == 3)),
                      r=[b_hT[i], bw[s2]], w=[bp] if k == 0 else (), cw=() if k == 0 else [bp])
                c.evac(ye[i][:, n4 * 512:(n4 + 1) * 512], ps[:], r=[bp], w=[b_ye[i]] if n4 == 0 else (), cw=() if n4 == 0 else [b_ye[i]])
            E("sp", lambda e, i=i, ex=ex: e.dma_start(out=ybuf[ex * CAP:(ex + 1) * CAP, :], in_=ye[i]), r=[b_ye[i]], w=[b_ybuf[ex]], dma=b_ybuf[ex])

    def combine(l, last, need_xT):
        bcast_load(gB[:], ln_g[l - L0, 1], b_gB, D)
        E("sp", lambda e: e.dma_start(out=bB[:], in_=ln_b[l - L0, 1].partition_broadcast(128)), cw=[b_gB], dma=b_gB)
        for t in range(T):
            i = t % 2
            xt, bx = xw[i], b_xw[i]
            E("sp", lambda e, xt=xt, t=t: e.dma_start(out=xt[:], in_=xm_d[t * 128:(t + 1) * 128, :]), r=[b_xm_d[t]], w=[bx], dma=bx)
            for n_ in range(2):
                E("pool", lambda e, i=i, n_=n_, t=t: e.indirect_dma_start(out=y01[i][:, n_, :], out_offset=None, in_=ybuf,
                                                                         in_offset=bass.IndirectOffsetOnAxis(ap=slots[:, t, n_:n_ + 1], axis=0)),
                  r=b_ybuf + [b_gs[t]], w=[b_y01[i]] if n_ == 0 else (), cw=() if n_ == 0 else [b_y01[i]], dma=b_y01[i])
            E("dve", lambda e, xt=xt: e.tensor_scalar(out=xt[:], in0=xt[:], scalar1=ALPHA, scalar2=None, op0=ALU.mult), w=[bx])
            for n_ in range(2):
                E("dve", lambda e, xt=xt, i=i, n_=n_, t=t: e.scalar_tensor_tensor(out=xt[:], in0=y01[i][:, n_, :], scalar=gates[:, t, n_:n_ + 1],
                                                                                 in1=xt[:], op0=ALU.mult, op1=ALU.add),
                  r=[b_y01[i], b_gs[t]], w=[bx])
            ln_tile(xt[:], bx, t, D, gB[:], bB[:], b_gB)
            if last:
                E("sp", lambda e, xt=xt, t=t: e.dma_start(out=y_out[t * 128:(t + 1) * 128, :], in_=xt[:]), r=[bx], w=[b_y[t]], dma=b_y[t])
            else:
                E("sp", lambda e, xt=xt, t=t: e.dma_start(out=x_d[t * 128:(t + 1) * 128, :], in_=xt[:]), r=[bx], w=[b_x_d[t]], dma=b_x_d[t])
            if need_xT:
                make_xT(xt, bx, t)

    def kv_project():
        Wkv = shared_w_kv
        ktb = catc[0:2]
        b_ktb = b_catc[0:2]
        vtb = c.vreg[:, 0:T * 6 * 257].rearrange("p (t h e) -> p t h e", t=T, h=6)
        for t in range(T):
            E("dve", lambda e, t=t: e.memset(vtb[:, t, :, 256:257], 1.0), w=[c.b_v[t]])
        n = 0
        for s in range(3):
            sl = load_slab_in(s % 2, Wkv, s * 512)
            for fc4 in range(4):
                fc = 4 * s + fc4
                for half in range(2):
                    ps, bp = c.next_ps()
                    for k in range(16):
                        E("pe", lambda e, ps=ps, k=k, fc4=fc4, half=half, sl=sl: e.matmul(
                            ps[:], lhsT=sl[:, k, fc4 * 128:(fc4 + 1) * 128], rhs=xT[:, k, half * 512:(half + 1) * 512],
                            start=(k == 0), stop=(k == 15)),
                          r=[bw[s % 2]] + b_xT[4 * half:4 * half + 4] + XG, w=[bp] if k == 0 else (), cw=() if k == 0 else [bp])
                    i = n % 2
                    n += 1
                    c.evac(ktb[i][:], ps[:], r=[bp], w=[b_ktb[i]])
                    E("sp", lambda e, i=i, fc=fc, half=half: e.dma_start(
                        out=kt_out[4 * half:4 * half + 4, fc // 2, :, fc % 2, :].rearrange("t d k -> d t k"),
                        in_=ktb[i][:].rearrange("p (t k) -> p t k", t=4)), r=[b_ktb[i]], cw=[c.b_kt], dma=c.b_kt)
        for s in range(3):
            sl = load_slab_in(s % 2, Wkv, 1536 + s * 512)
            for t in range(T):
                ps, bp = c.next_ps()
                for k in range(16):
                    E("pe", lambda e, ps=ps, k=k, t=t, sl=sl: e.matmul(ps[:], lhsT=xT[:, k, t * 128:(t + 1) * 128], rhs=sl[:, k, :],
                                                                      start=(k == 0), stop=(k == 15)),
                      r=[bw[s % 2], b_xT[t]] + XG, w=[bp] if k == 0 else (), cw=() if k == 0 else [bp])
                c.evac(vtb[:, t, 2 * s:2 * s + 2, 0:256], ps[:].rearrange("p (h e) -> p h e", h=2), r=[bp], cw=[c.b_v[t]])
        for t in range(T):
            if fused:
                E("sp", lambda e, t=t: e.dma_start(out=vl_v[t].rearrange("h k e -> k h e"), in_=vtb[:, t, :, :]), r=[c.b_v[t]],
                  w=[c.b_vout[t]], dma=c.b_vout[t])
            else:
                E("sp", lambda e, t=t: e.dma_start(out=v_out[t], in_=vtb[:, t, :, :]), r=[c.b_v[t]], w=[c.b_vout[t]], dma=c.b_vout[t])


    def build_tables():
        rb = xw[0][:, 0:192].rearrange("p (b h) -> p b h", b=32)
        E("sp", lambda e: e.dma_start(out=xw[0][:, 0:192], in_=rel_bias.rearrange("b h -> (b h)").partition_broadcast(128)),
          w=[b_xw[0]], dma=b_xw[0])
        rbs = xw[0][:, 256:448].rearrange("p (b h) -> p b h", b=32)
        E("dve", lambda e: e.tensor_tensor(out=rbs, in0=rb, in1=rb[:, 15:16, :].to_broadcast([128, 32, 6]), op=ALU.subtract), w=[b_xw[0]])
        E("dve", lambda e: e.tensor_scalar(out=rbs, in0=rbs, scalar1=1.0 / SCALE, scalar2=None, op0=ALU.mult), w=[b_xw[0]])
        bi = xw[1][:, 0:256]
        E("sp", lambda e: e.dma_start(out=bi, in_=bidx.rearrange("p a q -> p (a q)")), w=[b_xw[1]], dma=b_xw[1])
        acc = xw[1][:, 256:512]
        tmp = xw[1][:, 512:768]
        for h in range(6):
            E("dve", lambda e: e.tensor_scalar(out=acc, in0=bi, scalar1=32.0, scalar2=NEG, op0=ALU.is_equal, op1=ALU.mult), w=[b_xw[1]])
            for bk in range(32):
                E("dve", lambda e, bk=bk, h=h: e.tensor_scalar(out=tmp, in0=bi, scalar1=float(bk), scalar2=rbs[:, bk, h:h + 1],
                                                              op0=ALU.is_equal, op1=ALU.mult), r=[b_xw[0]], w=[b_xw[1]])
                E("dve", lambda e: e.tensor_tensor(out=acc, in0=acc, in1=tmp, op=ALU.add), w=[b_xw[1]])
            E("dve", lambda e, h=h: e.tensor_copy(out=c.tbl[:, h, :, :], in_=acc.rearrange("p (a q) -> p a q", a=2)), r=[b_xw[1]],
              w=[c.b_tbl] if h == 0 else (), cw=() if h == 0 else [c.b_tbl])
        E("sp", lambda e: e.dma_start(out=c.fmask[:], in_=farmask), w=[c.b_fmask], dma=c.b_fmask)

    def lam_params(i, l):
        lam_init = 0.8 - 0.6 * math.exp(-0.3 * l)
        lp = xw[0][:, 0:512]
        E("sp", lambda e: e.dma_start(out=lp, in_=b_lambda[i].rearrange("a d -> (a d)").partition_broadcast(128)), w=[b_xw[0]], dma=b_xw[0])
        pr = xw[0][:, 512:768]
        sc = c.lamt
        E("dve", lambda e: e.tensor_tensor(out=pr.rearrange("p (a d) -> p a d", a=2), in0=lp.rearrange("p (a d) -> p a d", a=4)[:, 0:4:2, :],
                                           in1=lp.rearrange("p (a d) -> p a d", a=4)[:, 1:4:2, :], op=ALU.mult), w=[b_xw[0]])
        E("dve", lambda e: e.tensor_reduce(out=sc[:, 0:2], in_=pr.rearrange("p (a d) -> p a d", a=2), axis=AX.X, op=ALU.add), r=[b_xw[0]], w=[c.b_lamt])
        E("act", lambda e: e.activation(out=sc[:, 2:4], in_=sc[:, 0:2], func=AF.Exp), w=[c.b_lamt])
        E("dve", lambda e: e.tensor_tensor(out=sc[:, 4:5], in0=sc[:, 2:3], in1=sc[:, 3:4], op=ALU.subtract), w=[c.b_lamt])
        E("dve", lambda e: e.tensor_scalar(out=sc[:, 5:6], in0=sc[:, 4:5], scalar1=lam_init, scalar2=-1.0, op0=ALU.add, op1=ALU.mult), w=[c.b_lamt])
        E("sp", lambda e: e.dma_start(out=c.gsub[:], in_=b_subln_g[i].partition_broadcast(128)), w=[c.b_gsub], dma=c.b_gsub)
        E("dve", lambda e: e.tensor_scalar(out=c.gsub[:], in0=c.gsub[:], scalar1=1.0 - lam_init, scalar2=None, op0=ALU.mult), w=[c.b_gsub])

    def attn_tile_head(t, h, qh, b_qh):
        nfar = LT[t] - 2
        chunks = [("near", 0, 2)] + [("far", j0, min(4, nfar - j0)) for j0 in range(0, nfar, 4)]
        oi = c.oi
        c.oi = (oi + 1) % 2
        O = [c.ps[(0, 6)[oi]], c.ps[(1, 7)[oi]]]
        bO = [c.bps[(0, 6)[oi]], c.bps[(1, 7)[oi]]]
        nsteps = 2 + nfar
        state = {"first_pv": True, "done": 0}

        def emit_pv(pr):
            (ks, vch, p0, pis) = pr
            for si in range(2):
                jj = p0 + si
                pt_ = c.ptring[pis[si]]
                state["done"] += 1
                last = (state["done"] == nsteps)
                fp = state["first_pv"]
                for m in range(2):
                    E("pe", lambda e, m=m, pt_=pt_, jj=jj, fp=fp, last=last: e.matmul(O[m][:, 0:257], lhsT=pt_[:, m * 128:(m + 1) * 128],
                                                                                     rhs=vch[:, jj, :], start=fp, stop=last),
                      r=[c.b_ptring[pis[si]], c.b_vring[ks]], w=[bO[m]] if fp else (), cw=() if fp else [bO[m]])
                state["first_pv"] = False

        pending = []
        for (ckind, j0, nb) in chunks:
            ks = c.kvi
            c.kvi = (ks + 1) % c.NKV
            kch, vch = c.kring[ks], c.vring[ks]
            if ckind == "near":
                ksrc = near_k[t, h]
                vsrc = near_v[t, h]
            else:
                ksrc = g_k[h, :, j0:j0 + nb]
                vsrc = g_v[h, :, j0:j0 + nb, :]
            E("sp", lambda e: e.dma_start(out=kch[:, 0:nb], in_=ksrc), w=[c.b_kring[ks]], dma=c.b_kring[ks])
            E("sp", lambda e: e.dma_start(out=vch[:, 0:nb, :], in_=vsrc), w=[c.b_vring[ks]], dma=c.b_vring[ks])
            for p0 in range(0, nb, 2):
                bank = 2 + c.sbank
                c.sbank = (c.sbank + 1) % 4
                ps, bp = c.ps[bank], c.bps[bank]
                for si in range(2):
                    jj = p0 + si
                    for m in range(2):
                        col = (2 * si + m) * 128
                        first = (si == 0 and m == 0)
                        E("pe", lambda e, col=col, jj=jj, m=m: e.matmul(ps[:, col:col + 128], lhsT=kch[:, jj, m, :],
                                                                        rhs=qh[:, m, t * 128:(t + 1) * 128],
                                                                        start=True, stop=(ckind != "near")),
                          r=[c.b_kring[ks], b_qh], w=[bp] if first else (), cw=() if first else [bp])
                        if ckind == "near":
                            E("pe", lambda e, col=col, jj=jj: e.matmul(ps[:, col:col + 128], lhsT=identb[:], rhs=c.tbl[:, h, jj, :],
                                                                     start=False, stop=True),
                              r=[b_cb, c.b_tbl], cw=[bp])
                pis = []
                for si in range(2):
                    jj = p0 + si
                    pi = c.pti
                    c.pti = (pi + 1) % 8
                    pis.append(pi)
                    pt_ = c.ptring[pi]
                    if ckind == "near":
                        E("act", lambda e, si=si, pt_=pt_: e.activation(out=pt_, in_=ps[:, si * 256:(si + 1) * 256], func=AF.Exp, scale=SCALE),
                          r=[bp], w=[c.b_ptring[pi]])
                    else:
                        E("act", lambda e, si=si, pt_=pt_, jj=jj: e.activation(out=pt_, in_=ps[:, si * 256:(si + 1) * 256], func=AF.Exp,
                                                                            scale=SCALE, bias=c.fmask[:, t, j0 + jj:j0 + jj + 1]),
                          r=[bp, c.b_fmask], w=[c.b_ptring[pi]])
                pending.append((ks, vch, p0, pis))
                if len(pending) > 1:
                    emit_pv(pending.pop(0))
        while pending:
            emit_pv(pending.pop(0))
        sc = c.asc[:, c.asi * 8:(c.asi + 1) * 8]
        bsc = c.b_asc[c.asi]
        c.asi = (c.asi + 1) % 4
        E("dve", lambda e: e.reciprocal(out=sc[:, 0:1], in_=O[0][:, 256:257]), r=[bO[0]], w=[bsc])
        E("dve", lambda e: e.reciprocal(out=sc[:, 1:2], in_=O[1][:, 256:257]), r=[bO[1]], w=[bsc])
        E("dve", lambda e: e.tensor_tensor(out=sc[:, 2:3], in0=sc[:, 1:2], in1=c.lamt[:, 5:6], op=ALU.mult), r=[c.b_lamt], w=[bsc])
        oi_ = c.ofi
        c.ofi = (oi_ + 1) % 2
        of, bof = c.of[oi_], c.b_of[oi_]
        E("dve", lambda e: e.tensor_scalar(out=of[:, 256:512], in0=O[1][:, 0:256], scalar1=sc[:, 2:3], scalar2=None, op0=ALU.mult),
          r=[bO[1], bsc], w=[bof])
        E("dve", lambda e: e.scalar_tensor_tensor(out=of[:, 0:256], in0=O[0][:, 0:256], scalar=sc[:, 0:1], in1=of[:, 256:512],
                                                 op0=ALU.mult, op1=ALU.add), r=[bO[0], bsc], w=[bof])
        E("dve", lambda e: e.scalar_tensor_tensor(out=of[:, 256:512], in0=of[:, 0:256], scalar=1.0, in1=of[:, 0:256],
                                                 op0=ALU.mult, op1=ALU.mult, accum_out=sc[:, 3:4]), r=[bsc], w=[bof])
        E("dve", lambda e: e.tensor_scalar(out=sc[:, 4:5], in0=sc[:, 3:4], scalar1=1.0 / 256.0, scalar2=EPS, op0=ALU.mult, op1=ALU.add),
          r=[bof], w=[bsc])
        E("pool", lambda e: e.tensor_tensor(out=sc[:, 6:7], in0=sc[:, 4:5], in1=cst[:, 417:418], op=ALU.pow), r=[b_cst], w=[bsc])
        ci = rot("catc", 3)
        cc, bcc = catc[ci], b_catc[ci]
        E("dve", lambda e: e.scalar_tensor_tensor(out=cc[:, 0:256], in0=of[:, 0:256], scalar=sc[:, 6:7], in1=c.gsub[:], op0=ALU.mult, op1=ALU.mult),
          r=[bof, bsc, c.b_gsub], w=[bcc])
        cat_chunk_to_catT(cc, bcc, 2 * h, t, n=2)

    def mixer_B(l):
        i = l - 2
        Wq = b_w_in[i]
        lam_params(i, l)
        sl = load_slab_in(1, Wq, 1536)
        q_featmajor(sl, bw[1], 4, lambda fc, half: qmT[:, fc, half * 512:(half + 1) * 512], b_qmT)
        mem_kv(l)
        for h in range(6):
            si = h % 2
            sl = load_slab_in(si, Wq, h * 256, ncols=256)
            qh = c.qh[h % 2]
            bqh = c.b_qh[h % 2]
            q_featmajor(sl, bw[si], 2, lambda fc, half, qh=qh: qh[:, fc, half * 512:(half + 1) * 512], bqh)
            for t in range(T):
                attn_tile_head(t, h, qh, bqh)
                if h == 2:
                    mem_attn_tile(t)
        return b_w_out[i]

    c.b_v = P.bufs(T, "vv")
    c.wsT = c.sb("wsT", [128, 4, 128], BF16)
    c.b_wsT = P.buf("wsT")
    c.bsT = c.sb("bsT", [128, 4], F32)
    c.b_bsT = P.buf("bsT")
    c.b_kt = P.buf("ktout")
    c.b_vout = P.bufs(T, "vout")
    P.share(c.b_vout)
    if mode in ("B", "F"):
        vr2 = c.vreg
        c.tbl = vr2[:, 0:1536].rearrange("p (h a q) -> p h a q", h=6, a=2)
        c.b_tbl = P.buf("tbl")
        c.NKV = 3
        o_k = 1536
        o_v = o_k + c.NKV * 1024
        o_p = o_v + c.NKV * 1028
        o_q = o_p + 8 * 256
        o_f = o_q + 2 * 2048
        assert o_f + 2048 <= 16896
        c.kring = [vr2[:, o_k + i * 1024:o_k + (i + 1) * 1024].rearrange("p (j m k) -> p j m k", j=4, m=2) for i in range(c.NKV)]
        c.vring = [vr2[:, o_v + i * 1028:o_v + (i + 1) * 1028].rearrange("p (j e) -> p j e", j=4) for i in range(c.NKV)]
        c.b_kring = P.bufs(c.NKV, "kr")
        c.b_vring = P.bufs(c.NKV, "vr")
        c.ptring = [vr2[:, o_p + i * 256:o_p + (i + 1) * 256] for i in range(8)]
        c.b_ptring = P.bufs(8, "ptr")
        c.qh = [vr2[:, o_q + i * 2048:o_q + (i + 1) * 2048].rearrange("p (m n) -> p m n", m=2) for i in range(2)]
        c.b_qh = P.bufs(2, "qh")
        c.of = [vr2[:, o_f + i * 1024:o_f + (i + 1) * 1024].bitcast(F32) for i in range(2)]
        c.b_of = P.bufs(2, "of")
        c.fmask = c.sb("fmask", [128, T, 64], F32)
        c.b_fmask = P.buf("fmask")
        c.lamt = c.sb("lamt", [128, 8], F32)
        c.b_lamt = P.buf("lamt")
        c.gsub = c.sb("gsub", [128, 256], F32)
        c.b_gsub = P.buf("gsub")
        c.asc = c.sb("asc", [128, 32], F32)
        c.b_asc = P.bufs(4, "asc")
        c.oi = c.sbank = c.kvi = c.pti = c.asi = c.ofi = 0
        if fused:
            c.pidx = c.sb("pidx", [128, T, 6], U32)
            c.nmask = c.sb("nmask", [128, T], F32)
            c.b_pidx = P.buf("pidx")
            c.b_gk = P.buf("gk")
            E("sp", lambda e: e.dma_start(out=c.pidx[:], in_=previdx), w=[c.b_pidx], dma=c.b_pidx)
            E("sp", lambda e: e.dma_start(out=c.nmask[:], in_=nearmask), cw=[c.b_pidx], dma=c.b_pidx)

    if mode == "F":
        for l in (0, 1):
            w_out = mixer_A(l)
            outproj_ln_route(l, w_out, x_in if l == 0 else x_d)
            experts(l)
            combine(l, last=False, need_xT=True)
        kv_project()
        rg = [list(range(NCORES))]
        E("pool", lambda e: e.collective_compute("AllGather", ALU.bypass, rg, ins=[ktl2], outs=[gk2]), r=[c.b_kt], w=[c.b_gk], dma=c.b_gk)
        E("pool", lambda e: e.collective_compute("AllGather", ALU.bypass, rg, ins=[vl2], outs=[gv2]), r=c.b_vout, cw=[c.b_gk], dma=c.b_gk)
        P.fence([c.b_gk])
        for l in (2, 3):
            build_tables()
            w_out = mixer_B(l)
            outproj_ln_route(l, w_out, x_d)
            experts(l)
            combine(l, last=(l == 3), need_xT=(l == 2))
        P.final_wait("sp", b_y)
    elif mode == "A":
        for l in layers:
            w_out = mixer_A(l)
            if debug and l == layers[-1]:
                b_dbg2 = P.bufs(2, "dbg2")
                E("sp", lambda e: e.dma_start(out=dbg_catT, in_=catT), r=b_catT + CG, w=[b_dbg2[0]], dma=b_dbg2[0])
                E("sp", lambda e: e.dma_start(out=dbg_vv, in_=c.vreg[:, 0:T * 1536]), r=c.b_v, w=[b_dbg2[1]], dma=b_dbg2[1])
            outproj_ln_route(l, w_out, x_in if l == 0 else x_d)
            experts(l)
            combine(l, last=False, need_xT=True)
            if debug and l == layers[-1]:
                b_dbg = P.bufs(5, "dbg")
                E("sp", lambda e: e.dma_start(out=dbg_slots, in_=slots[:]), r=b_gs, w=[b_dbg[0]], dma=b_dbg[0])
                E("sp", lambda e: e.dma_start(out=dbg_gates, in_=gates[:]), r=b_gs, w=[b_dbg[1]], dma=b_dbg[1])
                E("sp", lambda e: e.dma_start(out=dbg_xbuf, in_=xbuf), r=[b_xbuf], w=[b_dbg[2]], dma=b_dbg[2])
                E("sp", lambda e: e.dma_start(out=dbg_ybuf, in_=ybuf), r=b_ybuf, w=[b_dbg[3]], dma=b_dbg[3])
                E("sp", lambda e: e.dma_start(out=dbg_xm, in_=xm_d), r=b_xm_d, w=[b_dbg[4]], dma=b_dbg[4])
                P.final_wait("sp", b_dbg + b_dbg2)
        kv_project()
        for t in range(T):
            i = t % 2
            E("sp", lambda e, i=i, t=t: e.dma_start(out=xw[i][:], in_=x_d[t * 128:(t + 1) * 128, :]), r=[b_x_d[t]], w=[b_xw[i]], dma=b_xw[i])
            E("sp", lambda e, i=i, t=t: e.dma_start(out=y_out[t * 128:(t + 1) * 128, :], in_=xw[i][:]), r=[b_xw[i]], w=[b_y[t]], dma=b_y[t])
        P.final_wait("sp", b_y + [c.b_kt] + c.b_vout)
    else:
        tbl_d = c.dram("tbl_d", [128, 1536], BF16)
        b_tbl_d = P.buf("tbl_d")
        for l in layers:
            if l == layers[0]:
                build_tables()
                E("sp", lambda e: e.dma_start(out=tbl_d, in_=c.vreg[:, 0:1536]), r=[c.b_tbl], w=[b_tbl_d], dma=b_tbl_d)
            else:
                E("sp", lambda e: e.dma_start(out=c.vreg[:, 0:1536], in_=tbl_d), r=[b_tbl_d], w=[c.b_tbl, b_xe[0]], dma=c.b_tbl)
                E("sp", lambda e: e.dma_start(out=c.fmask[:], in_=farmask), w=[c.b_fmask], dma=c.b_fmask)
            w_out = mixer_B(l)
            if debug and l == 2:
                b_dbg3 = P.bufs(4, "dbg3")
                E("sp", lambda e: e.dma_start(out=dbg_catT, in_=catT), r=b_catT + CG, w=[b_dbg3[0]], dma=b_dbg3[0])
                E("sp", lambda e: e.dma_start(out=dbg_tbl, in_=c.vreg[:, 0:1536]), r=[c.b_tbl], w=[b_dbg3[1]], dma=b_dbg3[1])
                E("sp", lambda e: e.dma_start(out=dbg_qh, in_=c.vreg[:, 7184 + 2048:7184 + 4096]), r=c.b_qh, w=[b_dbg3[2]], dma=b_dbg3[2])
                E("sp", lambda e: e.dma_start(out=dbg_lam, in_=c.lamt[:]), r=[c.b_lamt], w=[b_dbg3[3]], dma=b_dbg3[3])
                P.final_wait("sp", b_dbg3)
            outproj_ln_route(l, w_out, x_in if l == 2 else x_d)
            experts(l)
            combine(l, last=(l == layers[-1]), need_xT=(l == 2 and len(layers) > 1))
        P.final_wait("sp", b_y)
    stats = P.finalize()
    st.close()
    return nc, stats


def make_consts(core):
    cst = np.zeros((128, 448), np.float32)
    cst[:, 0:128] = np.eye(128, dtype=np.float32)
    p = np.arange(128)
    cst[:, 128:256] = (p[:, None] < p[None, :]).astype(np.float32)
    cst[:, 256:384] = ((p[None, :] // 64) <= (p[:, None] // 64)).astype(np.float32)
    cst[:, 384:416] = (np.arange(32) * CAP)[None, :].astype(np.float32)
    cst[:, 416] = NSLOT + p
    cst[:, 417] = -0.5
    return cst


def shard_tokens(x2d, core):
    return np.ascontiguousarray(np.concatenate([x2d[b * 128:(b + 1) * 128] for b in GBLK[core]], axis=0))


def t5_bucket(rel):
    rel = np.asarray(rel, np.int64)
    n, max_exact = 16, 8
    ret = np.where(rel > 0, n, 0)
    a = np.abs(rel)
    af = np.maximum(a, 1).astype(np.float32)
    large = max_exact + (np.log(af / np.float32(max_exact)).astype(np.float32) / np.float32(math.log(128 / 8))
                         * np.float32(n - max_exact)).astype(np.int32)
    large = np.minimum(large, n - 1)
    return ret + np.where(a < max_exact, a, large)


def make_bidx():
    k = np.arange(128)[:, None]
    q = np.arange(128)[None, :]
    out = np.zeros((128, 2, 128), np.float32)
    diag = t5_bucket(k - q).astype(np.float32)
    diag[(k // 64) > (q // 64)] = 32.0
    out[:, 0, :] = diag
    out[:, 1, :] = t5_bucket(k - q - 128).astype(np.float32)
    return out


def make_farmask(core):
    fm = np.zeros((128, T, 64), np.float32)
    for t in range(T):
        b = GBLK[core][t]
        for j in range(64):
            if j > b - 2:
                fm[:, t, j] = NEG
    return fm


def make_near(core, kts, vs):
    nk = np.zeros((T, 6, 128, 2, 2, 128), kts[0].dtype)
    nv = np.zeros((T, 6, 128, 2, 257), vs[0].dtype)
    for t in range(T):
        b = GBLK[core][t]
        nk[t, :, :, 0] = kts[core][t]
        nv[t, :, :, 0] = np.transpose(vs[core][t], (1, 0, 2))
        if b >= 1:
            r, tt = OWNER[b - 1]
            nk[t, :, :, 1] = kts[r][tt]
            nv[t, :, :, 1] = np.transpose(vs[r][tt], (1, 0, 2))
    return nk, nv


def make_global_kv(kts, vs):
    gk = np.zeros((6, 128, 64, 2, 128), kts[0].dtype)
    gv = np.zeros((6, 128, 64, 257), vs[0].dtype)
    for r in range(NCORES):
        for t in range(T):
            j = GBLK[r][t]
            gk[:, :, j] = kts[r][t]
            gv[:, :, j] = np.transpose(vs[r][t], (1, 0, 2))
    return gk, gv


def _router_arrays(inputs, ls):
    wr = np.concatenate([inputs["moe_wg1"][ls], np.concatenate([inputs["moe_wg2"][ls, g] for g in range(4)], axis=-1)], axis=-1)
    br = np.concatenate([inputs["moe_bg1"][ls], inputs["moe_bg2"][ls].reshape(len(ls), 32)], axis=-1)
    return np.ascontiguousarray(wr, np.float32), np.ascontiguousarray(br, np.float32)


_PROGS = {}


def _prog(mode):
    if mode not in _PROGS:
        _PROGS[mode] = build_program(mode)[0]
    return _PROGS[mode]


def common_maps(inputs, ls):
    wr, br = _router_arrays(inputs, ls)
    m = {"mem": np.ascontiguousarray(inputs["mem"][0]), "ln_g": np.ascontiguousarray(inputs["ln_g"][ls]),
            "ln_b": np.ascontiguousarray(inputs["ln_b"][ls]), "mem_w_kv": np.ascontiguousarray(inputs["mem_w_kv"][ls]),
            "moe_wr": wr, "moe_br": br}
    for i in range(len(ls) // 2):
        sub = ls[2 * i:2 * i + 2]
        for nm in ("moe_w1", "moe_w3", "moe_w2"):
            m["%s_%d" % (nm, i)] = np.ascontiguousarray(inputs[nm][sub])
    return m


def maps_A(inputs, cores):
    cm = common_maps(inputs, [0, 1])
    x2d = np.asarray(inputs["x"][0], np.float32)
    out = []
    for cidx in cores:
        m = dict(cm)
        m.update({"x": shard_tokens(x2d, cidx), "consts": make_consts(cidx)})
        for k in ("a_w_in", "a_sgu_ln_g", "a_sgu_ln_b", "a_ws", "a_bs", "a_w_out", "shared_w_kv"):
            m[k] = np.ascontiguousarray(inputs[k], np.float32)
        out.append(m)
    return out


def maps_B(inputs, cores, xs, kts, vs):
    cm = common_maps(inputs, [2, 3])
    gk, gv = make_global_kv(kts, vs)
    bidx = make_bidx()
    out = []
    for n_, cidx in enumerate(cores):
        m = dict(cm)
        nk, nv = make_near(cidx, kts, vs)
        m.update({"x": xs[n_], "consts": make_consts(cidx), "g_k": gk, "g_v": gv, "near_k": nk, "near_v": nv,
                  "farmask": make_farmask(cidx), "bidx": bidx})
        for k in ("b_w_in", "b_lambda", "b_subln_g", "b_w_out", "rel_bias"):
            m[k] = np.ascontiguousarray(inputs[k], np.float32)
        out.append(m)
    return out


def make_prev(core):
    p = np.arange(128, dtype=np.int64)
    idx = np.zeros((128, T, 6), np.uint32)
    nm = np.zeros((128, T), np.float32)
    for t in range(T):
        b = GBLK[core][t]
        if b >= 1:
            r, tt = OWNER[b - 1]
        else:
            r, tt = core, t
            nm[:, t] = NEG
        for h_ in range(6):
            idx[:, t, h_] = ((r * T + tt) * 6 + h_) * 128 + p
    return idx, nm


def maps_F(inputs, cores):
    cm = common_maps(inputs, [0, 1, 2, 3])
    x2d = np.asarray(inputs["x"][0], np.float32)
    bidx = make_bidx()
    out = []
    for cidx in cores:
        m = dict(cm)
        pi, nm = make_prev(cidx)
        m.update({"x": shard_tokens(x2d, cidx), "consts": make_consts(cidx), "farmask": make_farmask(cidx), "bidx": bidx,
                  "previdx": pi, "nearmask": nm})
        for k in ("a_w_in", "a_sgu_ln_g", "a_sgu_ln_b", "a_ws", "a_bs", "a_w_out", "shared_w_kv",
                  "b_w_in", "b_lambda", "b_subln_g", "b_w_out", "rel_bias"):
            m[k] = np.ascontiguousarray(inputs[k], np.float32)
        out.append(m)
    return out


def kernel(**inputs):
    inputs = {k: np.asarray(v) for k, v in inputs.items()}
    cores = list(range(NCORES))
    resA = run_bass_kernel_spmd(_prog("A"), maps_A(inputs, cores), core_ids=cores).results
    xs = [np.asarray(r["y"], np.float32) for r in resA]
    kts = [np.asarray(r["kt"]) for r in resA]
    vs = [np.asarray(r["v"]) for r in resA]
    resB = run_bass_kernel_spmd(_prog("B"), maps_B(inputs, cores, xs, kts, vs), core_ids=cores).results
    out = np.zeros((8192, D), np.float32)
    for cidx in cores:
        y = np.asarray(resB[cidx]["y"], np.float32)
        for t in range(T):
            b = GBLK[cidx][t]
            out[b * 128:(b + 1) * 128] = y[t * 128:(t + 1) * 128]
    return out.reshape(1, 8192, D)
```
